# Optimizing a Trainium2 kernel written in Bass

```python
import math
import jax, jax.numpy as jnp
from jax import lax
import numpy as np

D_MODEL = 2048
BATCH = 32
SEQ = 256
DEPTH = 2
DEC_BATCH = 2
DEC_SEQ = 1024
PAST_LEN = 512

GRID_W = 64
N_EVEN = (DEPTH + 1) // 2
N_ODD = DEPTH // 2
NORM_EPS = 1e-6
NEG_INF = -1e30
NA_HEADS = 16
NA_HEAD_DIM = 64
NA_WIDTH = NA_HEADS * NA_HEAD_DIM
NA_WIN_ROWS = 8
NA_WIN_COLS = 16
NA_QC = 16
NA_KC = 32
CTX_Q_BLOCK = 128
HY_WIDTH = D_MODEL - NA_WIDTH
HY_BANDS = 16
HY_EMB = 1 + 2 * HY_BANDS
HY_FILT_HIDDEN = 64
HY_DECAY_TARGET = 1e-2
HY_SHORT_DECAY_PCT = 0.3
HY_LONG_DECAY_PCT = 1.5
HY_SHIFT = 0.05
FN_WIDTH = D_MODEL // 2
FN_GROUPS = 4
FN_GROUP = FN_WIDTH // FN_GROUPS
POOL_WINDOWS = (2, 4, 8, 16)
POOL_WIDTH = D_MODEL - FN_WIDTH
POOL_GROUP = POOL_WIDTH // len(POOL_WINDOWS)
MOE_GROUPS = 4
MOE_EXPERTS_PER_GROUP = 4
MOE_EXPERTS = MOE_GROUPS * MOE_EXPERTS_PER_GROUP
MOE_TOP_K = 2
MOE_FF = D_MODEL // 4

kernel_name = 'hybrid_natten_hyena_fnet_pool_hmoe_step'


def _rmsnorm(x, g):
    x32 = x.astype(jnp.float32)
    y = x32 * lax.rsqrt(jnp.mean(jnp.square(x32), axis=-1, keepdims=True) + NORM_EPS)
    return y.astype(x.dtype) * g


def _modulation(cvec, w, b):
    m = jax.nn.silu(cvec) @ w + b
    return [t[:, None, :] for t in jnp.split(m, 6, axis=-1)]


def _ctx_attention(q, k, v):
    b, lc, h, dh = q.shape
    nb = lc // CTX_Q_BLOCK
    qb = jnp.moveaxis(q.reshape(b, nb, CTX_Q_BLOCK, h, dh), 1, 0)
    scale = dh ** -0.5

    def block(qi):
        s = jnp.einsum('bqhd,bkhd->bhqk', qi, k).astype(jnp.float32) * scale
        p = jax.nn.softmax(s, axis=-1).astype(v.dtype)
        return jnp.einsum('bhqk,bkhd->bqhd', p, v)

    o = lax.map(block, qb)
    return jnp.moveaxis(o, 0, 1).reshape(b, lc, h * dh)


def _neigh_attention(q, k, v, k_ctx, v_ctx, rpb):
    b, seq_len, h, dh = q.shape
    rows = seq_len // GRID_W
    wr = min(NA_WIN_ROWS, rows)
    ncb = GRID_W // NA_QC
    nk = wr * NA_KC
    r = np.arange(rows)
    rs = np.clip(r - wr // 2, 0, rows - wr)
    key_rows = rs[:, None] + np.arange(wr)[None, :]
    j = np.arange(ncb)
    kc0 = np.clip(j * NA_QC - NA_WIN_COLS // 2, 0, GRID_W - NA_KC)
    key_cols = kc0[:, None] + np.arange(NA_KC)[None, :]
    q_cols = j[:, None] * NA_QC + np.arange(NA_QC)[None, :]
    wc0 = np.clip(q_cols - NA_WIN_COLS // 2, 0, GRID_W - NA_WIN_COLS)
    kcol = key_cols[:, None, :]
    col_ok = (kcol >= wc0[..., None]) & (kcol < wc0[..., None] + NA_WIN_COLS)
    mask = np.broadcast_to(col_ok[:, :, None, :], (ncb, NA_QC, wr, NA_KC)).reshape(ncb, 1, NA_QC, nk)
    dr = key_rows - r[:, None] + NA_WIN_ROWS - 1
    dc = np.clip(kcol - q_cols[..., None] + NA_WIN_COLS - 1, 0, 2 * NA_WIN_COLS - 2)
    bias = rpb[:, dr[:, None, None, :, None], dc[None, :, :, None, :]]
    bias = bias.reshape(h, rows, ncb, NA_QC, nk).transpose(1, 2, 0, 3, 4).astype(jnp.float32)
    qb = q.reshape(b, rows, ncb, NA_QC, h, dh)
    gr = key_rows[:, None, :, None]
    gc = key_cols[None, :, None, :]
    kg = k.reshape(b, rows, GRID_W, h, dh)[:, gr, gc].reshape(b, rows, ncb, nk, h, dh)
    vg = v.reshape(b, rows, GRID_W, h, dh)[:, gr, gc].reshape(b, rows, ncb, nk, h, dh)
    scale = dh ** -0.5
    s_loc = jnp.einsum('brjqhd,brjkhd->brjhqk', qb, kg).astype(jnp.float32) * scale + bias
    s_loc = jnp.where(mask, s_loc, NEG_INF)
    s_ctx = jnp.einsum('brjqhd,bkhd->brjhqk', qb, k_ctx).astype(jnp.float32) * scale
    p = jax.nn.softmax(jnp.concatenate([s_loc, s_ctx], axis=-1), axis=-1).astype(v.dtype)
    o = (jnp.einsum('brjhqk,brjkhd->brjqhd', p[..., :nk], vg)
         + jnp.einsum('brjhqk,bkhd->brjqhd', p[..., nk:], v_ctx))
    return o.reshape(b, seq_len, h * dh)


def _centred_conv3(u, w, b):
    up = jnp.pad(u, ((0, 0), (1, 1), (0, 0)))
    return up[:, :-2] * w[0] + up[:, 1:-1] * w[1] + up[:, 2:] * w[2] + b


def _hyena_filter(seq_len, w1, b1, freq, w2, b2, w3):
    t = jnp.linspace(0.0, 1.0, seq_len, dtype=jnp.float32)[:, None]
    omega = 2.0 * math.pi * jnp.arange(seq_len, dtype=jnp.float32)[:, None] / seq_len
    bands = jnp.linspace(1e-4, HY_BANDS - 1, HY_BANDS, dtype=jnp.float32)[None, :]
    z = jnp.concatenate([t, jnp.cos(bands * omega), -jnp.sin(bands * omega)], axis=-1)
    hdn = jnp.sin(freq * (z @ w1 + b1))
    hdn = jnp.sin(freq * (hdn @ w2 + b2))
    filt = (hdn @ w3).astype(jnp.float32).reshape(seq_len, 2, HY_WIDTH)
    max_decay = math.log(HY_DECAY_TARGET) / HY_SHORT_DECAY_PCT
    min_decay = math.log(HY_DECAY_TARGET) / HY_LONG_DECAY_PCT
    deltas = jnp.abs(jnp.linspace(min_decay, max_decay, HY_WIDTH, dtype=jnp.float32))
    window = jnp.exp(-t * deltas[None, :]) + HY_SHIFT
    filt = filt * window[:, None, :]
    fwd, bwd = filt[:, 0], filt[:, 1]
    k2 = jnp.concatenate([fwd, jnp.zeros((1, HY_WIDTH), jnp.float32), bwd[:0:-1]], axis=0)
    return k2 * lax.rsqrt(jnp.sum(jnp.square(k2), axis=0, keepdims=True) + NORM_EPS)


def _bidir_fftconv(u, k2):
    seq_len = u.shape[1]
    uf = jnp.fft.rfft(u.astype(jnp.float32), n=2 * seq_len, axis=1)
    kf = jnp.fft.rfft(k2, n=2 * seq_len, axis=0)
    return jnp.fft.irfft(uf * kf[None], n=2 * seq_len, axis=1)[:, :seq_len]


def _hyena(z, short_w, short_b, d_bias, w1, b1, freq, w2, b2, w3):
    z = _centred_conv3(z, short_w, short_b)
    v, x1, x0 = jnp.split(z, 3, axis=-1)
    u = v * x1
    k2 = _hyena_filter(z.shape[1], w1, b1, freq, w2, b2, w3)
    y = _bidir_fftconv(u, k2).astype(z.dtype) + d_bias * u
    return y * x0


def _even_proj(h, w_in):
    p = h @ w_in
    b, seq_len, _ = h.shape
    q, k, v = [t.reshape(b, seq_len, NA_HEADS, NA_HEAD_DIM) for t in jnp.split(p[..., :3 * NA_WIDTH], 3, axis=-1)]
    return q, k, v, p[..., 3 * NA_WIDTH:]


def _odd_mixer(h, w_in, fn_lin, pool_lin, pool_scale, w_out):
    p = h @ w_in
    b, seq_len, _ = p.shape
    uf = p[..., :FN_WIDTH].reshape(b, seq_len, FN_GROUPS, FN_GROUP).astype(jnp.float32)
    f = jnp.fft.fft2(uf, axes=(1, 3), norm='ortho').real.astype(p.dtype)
    f = jnp.einsum('blgc,gcd->blgd', f, fn_lin).reshape(b, seq_len, FN_WIDTH)
    up = p[..., FN_WIDTH:].astype(jnp.float32)
    cs = jnp.concatenate([jnp.zeros((b, 1, POOL_WIDTH), jnp.float32), jnp.cumsum(up, axis=1)], axis=1)
    pos = np.arange(seq_len)
    pooled = []
    for g, w in enumerate(POOL_WINDOWS):
        lo = np.clip(pos - w // 2, 0, seq_len)
        hi = np.clip(pos + w // 2, 0, seq_len)
        sl = slice(g * POOL_GROUP, (g + 1) * POOL_GROUP)
        cnt = (hi - lo).astype(np.float32)[None, :, None]
        pooled.append((cs[:, hi, sl] - cs[:, lo, sl]) / cnt - up[..., sl])
    pm = jnp.stack(pooled, axis=2).astype(p.dtype)
    pm = jnp.einsum('blgc,gcd->blgd', pm, pool_lin).reshape(b, seq_len, POOL_WIDTH) * pool_scale
    return jnp.concatenate([f, pm], axis=-1) @ w_out


def _hier_moe(h, w_group, b_group, w_expert, b_expert, w1, w3, w2):
    shape = h.shape
    t = h.reshape(-1, D_MODEL)
    n_tok = t.shape[0]
    g_prob = jax.nn.softmax((t @ w_group).astype(jnp.float32) + b_group, axis=-1)
    g_p, g_idx = lax.top_k(g_prob, 1)
    e_logits = ((t @ w_expert).astype(jnp.float32) + b_expert).reshape(n_tok, MOE_GROUPS, MOE_EXPERTS_PER_GROUP)
    e_sel = jnp.take_along_axis(e_logits, g_idx[:, :, None], axis=1)[:, 0]
    e_val, e_idx = lax.top_k(e_sel, MOE_TOP_K)
    e_w = jax.nn.softmax(e_val, axis=-1) * g_p
    ids = g_idx * MOE_EXPERTS_PER_GROUP + e_idx
    combine = jnp.einsum('tk,tke->te', e_w, jax.nn.one_hot(ids, MOE_EXPERTS, dtype=jnp.float32))
    hid = jax.nn.silu(jnp.einsum('td,edf->tef', t, w1)) * jnp.einsum('td,edf->tef', t, w3)
    hid = hid * combine[:, :, None].astype(hid.dtype)
    return jnp.einsum('tef,efd->td', hid, w2).reshape(shape)


def setup_inputs(seed: int = 0) -> dict:
    key = jax.random.key(seed)
    keys = iter(jax.random.split(key, 48))
    D = D_MODEL

    def nrm(shape, scale):
        return jax.random.normal(next(keys), shape, jnp.float32) * scale

    return {
        'x_prompt': nrm((BATCH, SEQ, D), 1.0),
        'x_sample': nrm((DEC_BATCH, DEC_SEQ, D), 1.0),
        'c': nrm((DEC_BATCH, D), 1.0),
        'cache_k': nrm((DEC_BATCH, N_EVEN, PAST_LEN, NA_HEADS, NA_HEAD_DIM), 1.0),
        'cache_v': nrm((DEC_BATCH, N_EVEN, PAST_LEN, NA_HEADS, NA_HEAD_DIM), 1.0),
        'c_ctx': nrm((D,), 1.0),
        'ada_w': nrm((DEPTH, D, 6 * D), 0.5 * D ** -0.5),
        'ada_b': nrm((DEPTH, 6 * D), 0.02),
        'norm_mix': 1.0 + nrm((DEPTH, D), 0.1),
        'norm_ffn': 1.0 + nrm((DEPTH, D), 0.1),
        'norm_final': 1.0 + nrm((D,), 0.1),
        'w_in_even': nrm((N_EVEN, D, 3 * NA_WIDTH + 3 * HY_WIDTH), D ** -0.5),
        'w_out_even': nrm((N_EVEN, D, D), D ** -0.5),
        'na_rpb': nrm((N_EVEN, NA_HEADS, 2 * NA_WIN_ROWS - 1, 2 * NA_WIN_COLS - 1), 0.1),
        'hy_short_w': nrm((N_EVEN, 3, 3 * HY_WIDTH), 3 ** -0.5),
        'hy_short_b': nrm((N_EVEN, 3 * HY_WIDTH), 0.02),
        'hy_filt_w1': nrm((N_EVEN, HY_EMB, HY_FILT_HIDDEN), HY_EMB ** -0.5),
        'hy_filt_b1': nrm((N_EVEN, HY_FILT_HIDDEN), 0.1),
        'hy_filt_freq': 1.0 + nrm((N_EVEN, HY_FILT_HIDDEN), 0.1),
        'hy_filt_w2': nrm((N_EVEN, HY_FILT_HIDDEN, HY_FILT_HIDDEN), HY_FILT_HIDDEN ** -0.5),
        'hy_filt_b2': nrm((N_EVEN, HY_FILT_HIDDEN), 0.1),
        'hy_filt_w3': nrm((N_EVEN, HY_FILT_HIDDEN, 2 * HY_WIDTH), HY_FILT_HIDDEN ** -0.5),
        'hy_bias_d': nrm((N_EVEN, HY_WIDTH), 0.5),
        'w_in_odd': nrm((N_ODD, D, FN_WIDTH + POOL_WIDTH), D ** -0.5),
        'fn_lin': nrm((N_ODD, FN_GROUPS, FN_GROUP, FN_GROUP), FN_GROUP ** -0.5),
        'pool_lin': nrm((N_ODD, len(POOL_WINDOWS), POOL_GROUP, POOL_GROUP), POOL_GROUP ** -0.5),
        'pool_scale': 1.0 + nrm((N_ODD, POOL_WIDTH), 0.1),
        'w_out_odd': nrm((N_ODD, D, D), D ** -0.5),
        'moe_w_group': nrm((DEPTH, D, MOE_GROUPS), D ** -0.5),
        'moe_b_group': nrm((DEPTH, MOE_GROUPS), 0.01),
        'moe_w_expert': nrm((DEPTH, D, MOE_EXPERTS), D ** -0.5),
        'moe_b_expert': nrm((DEPTH, MOE_EXPERTS), 0.01),
        'moe_w1': nrm((DEPTH, MOE_EXPERTS, D, MOE_FF), D ** -0.5),
        'moe_w3': nrm((DEPTH, MOE_EXPERTS, D, MOE_FF), D ** -0.5),
        'moe_w2': nrm((DEPTH, MOE_EXPERTS, MOE_FF, D), MOE_FF ** -0.5),
    }


def reference(x_prompt, x_sample, c, cache_k, cache_v, c_ctx, ada_w, ada_b, norm_mix, norm_ffn, norm_final,
              w_in_even, w_out_even, na_rpb, hy_short_w, hy_short_b, hy_filt_w1, hy_filt_b1, hy_filt_freq,
              hy_filt_w2, hy_filt_b2, hy_filt_w3, hy_bias_d, w_in_odd, fn_lin, pool_lin, pool_scale, w_out_odd,
              moe_w_group, moe_b_group, moe_w_expert, moe_b_expert, moe_w1, moe_w3, moe_w2):
    xp, xs = x_prompt, x_sample
    new_k, new_v = [], []
    for l in range(DEPTH):
        mp = _modulation(c_ctx[None, :], ada_w[l], ada_b[l])
        ms = _modulation(c, ada_w[l], ada_b[l])
        hp = _rmsnorm(xp, norm_mix[l]) * (1.0 + mp[1]) + mp[0]
        hs = _rmsnorm(xs, norm_mix[l]) * (1.0 + ms[1]) + ms[0]
        if l % 2 == 0:
            e = l // 2
            filt = (hy_filt_w1[e], hy_filt_b1[e], hy_filt_freq[e], hy_filt_w2[e], hy_filt_b2[e], hy_filt_w3[e])
            qp, kp, vp, zp = _even_proj(hp, w_in_even[e])
            ap = _ctx_attention(qp, kp, vp)
            bp = _hyena(zp, hy_short_w[e], hy_short_b[e], hy_bias_d[e], *filt)
            yp = jnp.concatenate([ap, bp], axis=-1) @ w_out_even[e]
            new_k.append(kp)
            new_v.append(vp)
            qs, ks, vs, zs = _even_proj(hs, w_in_even[e])
            a_s = _neigh_attention(qs, ks, vs, cache_k[:, e], cache_v[:, e], na_rpb[e])
            b_s = _hyena(zs, hy_short_w[e], hy_short_b[e], hy_bias_d[e], *filt)
            ys = jnp.concatenate([a_s, b_s], axis=-1) @ w_out_even[e]
        else:
            o = l // 2
            yp = _odd_mixer(hp, w_in_odd[o], fn_lin[o], pool_lin[o], pool_scale[o], w_out_odd[o])
            ys = _odd_mixer(hs, w_in_odd[o], fn_lin[o], pool_lin[o], pool_scale[o], w_out_odd[o])
        xp = xp + mp[2] * yp
        xs = xs + ms[2] * ys
        moe = (moe_w_group[l], moe_b_group[l], moe_w_expert[l], moe_b_expert[l], moe_w1[l], moe_w3[l], moe_w2[l])
        hp = _rmsnorm(xp, norm_ffn[l]) * (1.0 + mp[4]) + mp[3]
        hs = _rmsnorm(xs, norm_ffn[l]) * (1.0 + ms[4]) + ms[3]
        xp = xp + mp[5] * _hier_moe(hp, *moe)
        xs = xs + ms[5] * _hier_moe(hs, *moe)
    y_prompt = _rmsnorm(xp, norm_final)
    y_sample = _rmsnorm(xs, norm_final)
    new_cache_k = jnp.stack(new_k, axis=1)
    new_cache_v = jnp.stack(new_v, axis=1)
    return (y_prompt, y_sample, new_cache_k, new_cache_v)
```

```python
import math
from contextlib import ExitStack

import numpy as np
import ml_dtypes

import concourse.bass as bass
import concourse.mybir as mybir
from concourse.bass_utils import run_bass_kernel_spmd

F32 = mybir.dt.float32
BF16 = mybir.dt.bfloat16
I32 = mybir.dt.int32
AF = mybir.ActivationFunctionType
ALU = mybir.AluOpType
AX = mybir.AxisListType

D = 2048
NCORES = 8
SEQ = 256
DSEQ = 1024
PAST = 512
NH = 16
HD = 64
NAW = 1024
HYW = 1024
GRID_W = 64
EPS = 1e-6
NEXP = 16
FF = 512

ENG = ("pe", "act", "dve", "pool", "sp")


class Op:
    __slots__ = ("eng", "fn", "deps", "needs_inc", "sem", "val", "dma", "slot")

    def __init__(self, eng, fn, dma):
        self.eng = eng
        self.fn = fn
        self.deps = set()
        self.needs_inc = False
        self.sem = None
        self.val = 0
        self.dma = dma
        self.slot = None


class Sched:
    def __init__(self, nc, ring=14):
        self.nc = nc
        self.ops = {e: [] for e in ENG}
        self.last_w = {}
        self.readers = {}
        self.ring = ring
        self.ring_last = {q: [None] * ring for q in ("sp", "pool", "act")}
        self.ring_next = {q: 0 for q in ("sp", "pool", "act")}
        self.ring_cnt = {q: [0] * ring for q in ("sp", "pool", "act")}
        self.live_dma = []
        self.n = 0

    def rec(self, eng, fn, reads=(), writes=(), dma=False, extra=()):
        op = Op(eng, fn, dma)
        deps = set(extra)
        for k in reads:
            w = self.last_w.get(k)
            if w is not None:
                deps.add(w)
            if isinstance(k, tuple) and k[0] == "ps":
                for r in self.readers.get(k, ()):
                    if r.eng != eng:
                        deps.add(r)
        for k in writes:
            w = self.last_w.get(k)
            if w is not None:
                deps.add(w)
            for r in self.readers.get(k, ()):
                deps.add(r)
        if dma:
            q = eng
            i = self.ring_next[q]
            self.ring_next[q] = (i + 1) % self.ring
            prev = self.ring_last[q][i]
            if prev is not None:
                deps.add(prev)
            self.ring_last[q][i] = op
            self.ring_cnt[q][i] += 1
            op.slot = (q, i)
            op.val = 16 * self.ring_cnt[q][i]
            op.needs_inc = True
            self.live_dma.append(op)
        deps.discard(op)
        if eng == "pe" and not dma:
            deps = {d for d in deps if not (d.eng == "pe" and not d.dma)}
        for d in deps:
            d.needs_inc = True
        op.deps = deps
        for k in reads:
            self.readers.setdefault(k, []).append(op)
        for k in writes:
            self.last_w[k] = op
            self.readers[k] = []
        self.ops[eng].append(op)
        self.n += 1
        return op

    def barrier(self):
        lasts = [self.ops[e][-1] for e in ENG if self.ops[e]]
        extra = set(lasts) | set(self.live_dma)
        for e in ENG:
            self.rec(e, None, extra=extra)
        self.live_dma = []
        self.last_w = {}
        self.readers = {}

    def emit(self, es):
        nc = self.nc
        esem = {e: es.enter_context(nc.semaphore("s_" + e)) for e in ENG}
        dsem = {}
        for q in ("sp", "pool", "act"):
            if any(c > 0 for c in self.ring_cnt[q]):
                for i in range(self.ring):
                    if self.ring_cnt[q][i] > 0:
                        dsem[(q, i)] = es.enter_context(nc.semaphore(f"d_{q}{i}"))
        for e in ENG:
            c = 0
            for op in self.ops[e]:
                if op.dma:
                    op.sem = dsem[op.slot]
                elif op.needs_inc and op.fn is not None:
                    c += 1
                    op.sem = esem[e]
                    op.val = c
                else:
                    op.sem = None
        block = es.enter_context(nc.Block())

        def run(e, eng):
            waited = {}
            for op in self.ops[e]:
                for d in op.deps:
                    if d.sem is None:
                        continue
                    key = id(d.sem)
                    if waited.get(key, 0) < d.val:
                        eng.wait_ge(d.sem, d.val)
                        waited[key] = d.val
                if op.fn is None:
                    continue
                inst = op.fn(eng)
                if op.dma:
                    inst.then_inc(op.sem, 16)
                elif op.sem is not None:
                    inst.then_inc(op.sem, 1)

        @block.tensor
        def _(t):
            run("pe", t)

        @block.scalar
        def _(a):
            run("act", a)

        @block.vector
        def _(v):
            run("dve", v)

        @block.gpsimd
        def _(g):
            run("pool", g)

        @block.sync
        def _(s):
            run("sp", s)


class Arena:
    def __init__(self, handle, words):
        self.h = handle
        self.words = words
        self.off = 0

    def alloc(self, free_shape, dtype=F32, parts=128):
        n = int(np.prod(free_shape))
        bpe = 2 if dtype == BF16 else 4
        w = (n * bpe + 3) // 4
        w = (w + 7) // 8 * 8
        assert self.off + w <= self.words, f"arena overflow {self.off}+{w}>{self.words}"
        ap = self.h[:, self.off:self.off + w]
        self.off += w
        if dtype != F32:
            ap = ap.bitcast(dtype)
        ap = ap[:, 0:n]
        if len(free_shape) == 2:
            ap = ap.rearrange("p (a b) -> p a b", a=free_shape[0])
        elif len(free_shape) == 3:
            ap = ap.rearrange("p (a b c) -> p a b c", a=free_shape[0], b=free_shape[1])
        elif len(free_shape) == 4:
            ap = ap.rearrange("p (a b c d) -> p a b c d", a=free_shape[0], b=free_shape[1], c=free_shape[2])
        if parts != 128:
            ap = ap[0:parts]
        return ap


class K:
    def __init__(self, nc, dbg=False):
        self.nc = nc
        self.dbg = dbg
        self.s = Sched(nc)
        self.inputs = {}
        self.outputs = {}
        self.scr = {}
        self.uid = 0

    def inp(self, name, shape, dtype=F32):
        if name not in self.inputs:
            self.inputs[name] = self.nc.dram_tensor(name, list(shape), dtype, kind="ExternalInput").ap()
        return self.inputs[name]

    def out(self, name, shape, dtype=F32):
        if name not in self.outputs:
            self.outputs[name] = self.nc.dram_tensor(name, list(shape), dtype, kind="ExternalOutput").ap()
        return self.outputs[name]

    def scratch(self, name, shape, dtype=F32):
        if name not in self.scr:
            if self.dbg:
                self.scr[name] = self.out(name, shape, dtype)
            else:
                self.scr[name] = self.nc.dram_tensor(name, list(shape), dtype).ap()
        return self.scr[name]

    def dma(self, out, in_, reads=(), writes=(), q="sp"):
        return self.s.rec(q, lambda e: e.dma_start(out=out, in_=in_), reads=reads, writes=writes, dma=True)

    def gather(self, out, in_rows, idx, reads=(), writes=()):
        return self.s.rec("pool", lambda e: e.indirect_dma_start(out=out, out_offset=None, in_=in_rows,
                                                                 in_offset=bass.IndirectOffsetOnAxis(ap=idx, axis=0)),
                          reads=reads, writes=writes, dma=True)

    def mm(self, ps, lhsT, rhs, start, stop, reads, writes, tr=False, sgc=False):
        if tr:
            fn = lambda e: e.matmul(ps, lhsT=lhsT, rhs=rhs, is_transpose=True)
        elif sgc:
            fn = lambda e: e.matmul(ps, lhsT=lhsT, rhs=rhs, start=start, stop=stop, skip_group_check=True)
        else:
            fn = lambda e: e.matmul(ps, lhsT=lhsT, rhs=rhs, start=start, stop=stop)
        return self.s.rec("pe", fn, reads=reads, writes=writes)

    def act(self, out, in_, func, reads, writes, bias=None, scale=None, accum_out=None):
        kw = {}
        if bias is not None:
            kw["bias"] = bias
        if scale is not None:
            kw["scale"] = scale
        if accum_out is not None:
            kw["accum_out"] = accum_out
        return self.s.rec("act", lambda e: e.activation(out=out, in_=in_, func=func, **kw), reads=reads, writes=writes)

    def tt(self, out, in0, in1, op, reads, writes, eng="dve"):
        return self.s.rec(eng, lambda e: e.tensor_tensor(out=out, in0=in0, in1=in1, op=op), reads=reads, writes=writes)

    def ts(self, out, in0, s1, op0, reads, writes, s2=None, op1=None, eng="dve"):
        if op1 is None:
            fn = lambda e: e.tensor_scalar(out=out, in0=in0, scalar1=s1, scalar2=None, op0=op0)
        else:
            fn = lambda e: e.tensor_scalar(out=out, in0=in0, scalar1=s1, scalar2=s2, op0=op0, op1=op1)
        return self.s.rec(eng, fn, reads=reads, writes=writes)

    def stt(self, out, in0, scalar, in1, op0, op1, reads, writes):
        return self.s.rec("dve", lambda e: e.scalar_tensor_tensor(out=out, in0=in0, scalar=scalar, in1=in1, op0=op0, op1=op1),
                          reads=reads, writes=writes)

    def copy(self, out, in_, reads, writes, eng="dve"):
        if eng == "act":
            return self.s.rec("act", lambda e: e.copy(out=out, in_=in_), reads=reads, writes=writes)
        return self.s.rec(eng, lambda e: e.tensor_copy(out=out, in_=in_), reads=reads, writes=writes)

    def memset(self, ap, val, writes, eng="pool"):
        return self.s.rec(eng, lambda e: e.memset(ap, val), writes=writes)

    def recip(self, out, in_, reads, writes):
        return self.s.rec("dve", lambda e: e.reciprocal(out=out, in_=in_), reads=reads, writes=writes)


def setup_persist(k):
    A = k.arena
    P = {}
    P["ones_bf"] = A.alloc([128], BF16)
    P["ones_f"] = A.alloc([128])
    P["ident"] = A.alloc([128])
    P["eps"] = A.alloc([1])
    P["modT"] = A.alloc([2, 2, 96])
    P["normT"] = A.alloc([5, 16])
    P["Amod"] = A.alloc([2, 2, 2, 16])
    k.memset(P["ones_bf"], 1.0, writes=["ones"])
    k.memset(P["ones_f"], 1.0, writes=["ones_f"])
    k.memset(P["eps"], EPS, writes=["eps"])
    k.memset(P["ident"], 0.0, writes=["ident"])
    idn = P["ident"]
    k.s.rec("pool", lambda e: e.affine_select(out=idn, in_=idn, pattern=[[-1, 128]], compare_op=ALU.not_equal,
                                             fill=1.0, base=0, channel_multiplier=1), reads=["ident"], writes=["ident"])
    P["own_idx"] = A.alloc([16], I32)
    k.P = P
    return P


def mod_views(k, l, which, j):
    P = k.P
    m = P["modT"]
    base = which * 3
    A_ = P["Amod"][:, l, which, j, :]
    B_ = m[:, l, j, (base + 0) * 16:(base + 1) * 16]
    G_ = m[:, l, j, (base + 2) * 16:(base + 3) * 16]
    return A_, B_, G_


def mod_evac(k, bT, l, ps3, ch0, ch1, whiches):
    P = k.P
    for j in range(2):
        k.tt(P["modT"][:, l, j, ch0:ch1], ps3[:, ch0:ch1, j], bT[:, l, ch0:ch1], ALU.add, reads=[("modps", l, ch0), "bT"],
             writes=[("modT", l, j)])
        for which in whiches:
            sc = P["modT"][:, l, j, (which * 3 + 1) * 16:(which * 3 + 2) * 16]
            nrm = P["normT"][:, (2 * which + l), :]
            k.stt(P["Amod"][:, l, which, j, :], sc, 1.0, nrm, ALU.add, ALU.mult,
                  reads=[("modT", l, j), "normT"], writes=[("Amod", l, which, j)])


def phase_mod(k):
    A = k.arena
    P = k.P
    ada_w = k.inp("ada_w", [2, D, 6 * D])
    cT = k.inp("cT", [128, 16, 2])
    ada_bT = k.inp("ada_bT", [128, 2, 96])
    normT = k.inp("normT", [128, 5, 16])
    c32 = A.alloc([16, 2])
    sT = A.alloc([16, 2], BF16)
    bT = A.alloc([2, 96])
    k.dma(c32, cT, writes=["c32"])
    k.dma(bT, ada_bT, writes=["bT"])
    k.dma(P["normT"], normT, writes=["normT"])
    k.act(sT, c32, AF.Silu, reads=["c32"], writes=["sT"])
    NB = 3
    wb = [A.alloc([16, 512], BF16) for _ in range(NB)]
    cnt = [0]

    def block(l, cb, ps3, key):
        wv = ada_w[l].rearrange("(k p) c -> p k c", p=128)
        i = cnt[0] % NB
        cnt[0] += 1
        k.dma(wb[i], wv[:, :, cb * 512:(cb + 1) * 512], writes=[("mwb", i)], q="pool")
        for fc in range(4):
            ch = cb * 4 + fc
            for kk in range(16):
                k.mm(ps3[:, ch, :], wb[i][:, kk, fc * 128:(fc + 1) * 128], sT[:, kk, :], kk == 0, kk == 15,
                     reads=[("mwb", i), "sT"], writes=[key])

    def view(b):
        return k.ps[b][:, 0:192].rearrange("p (c j) -> p c j", j=2)

    for cb in range(8):
        block(0, cb, view(4), ("modps", 0, 0))
    mod_evac(k, bT, 0, view(4), 0, 32, [0])

    def gen():
        for cb in range(8, 24):
            block(0, cb, view(6), ("modps", 0, 32))
            yield
        mod_evac(k, bT, 0, view(6), 32, 96, [1])
        for cb in range(24):
            block(1, cb, view(7), ("modps", 1, 0))
            yield
        mod_evac(k, bT, 1, view(7), 0, 96, [0, 1])

    k.bg = gen()


def tick(k):
    if getattr(k, "bg", None) is not None:
        try:
            next(k.bg)
        except StopIteration:
            k.bg = None


def phase_prenorm(k, src, T, l, which, cond_of_tb, hT, hkey, hf_cb=None, nbuf=2, bw=512):
    A = k.arena
    P = k.P
    mark = A.off
    xv = src.rearrange("(k p) t -> p k t", p=128)
    xb = [A.alloc([16, bw]) for _ in range(nbuf)]
    if nbuf == 1:
        xb = [xb[0], xb[0]]
    sq = A.alloc([16, bw], BF16)
    rs = A.alloc([bw])
    hf = None
    nb = T // bw
    for tb in range(nb):
        b = tb % nbuf
        tbk = (tb * bw) // 512
        k.dma(xb[b], xv[:, :, tb * bw:(tb + 1) * bw], writes=[("xb", b)])
        k.act(sq, xb[b], AF.Square, reads=[("xb", b)], writes=["sq"])
        pb = 2 + tb % 2
        ps = k.ps[pb][:, 0:bw]
        for kk in range(16):
            k.mm(ps, P["ones_bf"], sq[:, kk, :], kk == 0, kk == 15, reads=["sq", "ones"], writes=[("ps", pb)])
        k.act(rs, ps, AF.Sqrt, bias=P["eps"], scale=1.0 / D, reads=[("ps", pb), "eps"], writes=["rs"])
        k.recip(rs, rs, reads=["rs"], writes=["rs"])
        j = cond_of_tb(tbk)
        A_, B_, _ = mod_views(k, l, which, j)
        k.tt(xb[b], xb[b], rs.unsqueeze(1).to_broadcast([128, 16, bw]), ALU.mult, reads=[("xb", b), "rs"],
             writes=[("xb", b)])
        for kk in range(16):
            k.act(hT[:, kk, tb * bw:(tb + 1) * bw], xb[b][:, kk, :], AF.Identity, bias=B_[:, kk:kk + 1],
                  scale=A_[:, kk:kk + 1], reads=[("xb", b), ("modT", l, j), ("Amod", l, which, j)], writes=[(hkey, tbk, kk)])
        if hf_cb is not None:
            hf_cb(tb, hf)
    k.s.barrier()
    A.off = mark


ARENA_WORDS = 52992


def new_builder(dbg=False):
    nc = bass.Bass("TRN2", target_bir_lowering=False)
    k = K(nc, dbg)
    es = ExitStack()
    arena_h = es.enter_context(nc.sbuf_tensor("arena", [128, ARENA_WORDS], F32))
    k.arena = Arena(arena_h, ARENA_WORDS)
    k.psall = es.enter_context(nc.psum_tensor("psall", [128, 4096], F32))
    k.ps = [k.psall[:, i * 512:(i + 1) * 512] for i in range(8)]
    k.es = es
    setup_persist(k)
    return k


def finish(k):
    k.s.barrier()
    k.s.emit(k.es)
    k.es.close()
    return k.nc


def pk(v):
    v = np.asarray(v)
    lead = v.shape[:-1]
    n = v.shape[-1] // 128
    r = v.reshape(*lead, n, 128)
    return np.ascontiguousarray(np.moveaxis(r, -1, 0))


def prep_core_inputs(inp, core):
    b = core // 4
    d = {}
    xp = inp["x_prompt"][core * 4:(core + 1) * 4].reshape(4 * SEQ, D)
    xs = inp["x_sample"][b]
    d["xT"] = np.ascontiguousarray(np.concatenate([xp, xs], axis=0).T)
    cv = np.stack([inp["c_ctx"], inp["c"][b]], axis=0)
    d["cT"] = np.ascontiguousarray(np.transpose(cv.reshape(2, 16, 128), (2, 1, 0)))
    d["ada_w"] = inp["ada_w"]
    d["ada_bT"] = pk(inp["ada_b"])
    nv = np.stack([inp["norm_mix"][0], inp["norm_mix"][1], inp["norm_ffn"][0], inp["norm_ffn"][1],
                   inp["norm_final"]], axis=0)
    d["normT"] = pk(nv)
    r = core % 4
    d["own_idx"] = ((np.arange(16)[None, :] * 128 + np.arange(128)[:, None]) * 8 + 4 + r).astype(np.int32)
    return d


def na_geometry():
    rows = DSEQ // GRID_W
    rs = np.clip(np.arange(rows) - 4, 0, rows - 8)
    rowok = np.zeros((rows, rows), bool)
    for qr in range(rows):
        rowok[rs[qr]:rs[qr] + 8, qr] = True
    chunks = []
    for c in range(8):
        ok = rowok[2 * c] | rowok[2 * c + 1]
        q = np.nonzero(ok)[0]
        qlo, qhi = int(q.min()), int(q.max())
        qlo -= qlo % 2
        qhi += 1 - (qhi % 2)
        chunks.append((qlo, qhi))
    return rowok, chunks


def natt_table(rpb):
    kc = np.arange(64)[:, None]
    qc = np.arange(64)[None, :]
    wc0 = np.clip(qc - 8, 0, 48)
    colok = (kc >= wc0) & (kc < wc0 + 16)
    dc = np.clip(kc - qc + 15, 0, 30)
    out = np.full((2, 64, NH, 14, 64), -1e30, np.float32)
    for krl in range(2):
        for j in range(14):
            dr = krl + 13 - j
            g = rpb[:, dr][:, dc]
            g = np.where(colok[None], g, np.float32(-1e30))
            out[krl, :, :, j, :] = np.transpose(g, (1, 0, 2))
    return np.ascontiguousarray(out.reshape(128, NH, 14, 64))


def phase_inproj0(k, hT, T):
    A = k.arena
    mark = A.off
    w = k.inp("w_in_even", [1, D, 6144])
    wv = w[0].rearrange("(k p) c -> p k c", p=128)
    pT_d = k.scratch("pT_d", [5120, T], BF16)
    v_tok = k.scratch("v_tok", [T, 1024], BF16)
    nkT = k.out("nkT", [1024, 1024])
    nv = k.out("nv", [1024, 1024])
    NB = 3
    wb = [A.alloc([16, 512], BF16) for _ in range(NB)]
    ob = [A.alloc([T], BF16) for _ in range(2)]
    okf = [A.alloc([1024]) for _ in range(2)]
    n = 0
    nps = 0
    hkeys = [[("hT", tb, kk) for kk in range(16)] for tb in range(T // 512)]
    groups = [(0, 8, 0), (1024, 8, 1024), (3072, 24, 2048)]
    import os
    if os.environ.get("DBG_GROUPS"):
        groups = [groups[int(c)] for c in os.environ["DBG_GROUPS"] if c.isdigit()]
    nchunk = 0
    for (c0, nch, r0) in groups:
        for cb in range(nch // 4):
            i = n % NB
            n += 1
            k.dma(wb[i], wv[:, :, c0 + cb * 512:c0 + (cb + 1) * 512], writes=[("wb", i)], q="pool")
            for fc in range(4):
                ch = cb * 4 + fc
                o = nchunk % 2
                nchunk += 1
                is_k = (c0 == 1024)
                for tb in range(T // 512):
                    b = nps % 4
                    nps += 1
                    ps = k.ps[b]
                    for kk in range(16):
                        k.mm(ps, wb[i][:, kk, fc * 128:(fc + 1) * 128], hT[:, kk, tb * 512:(tb + 1) * 512],
                             kk == 0, kk == 15, reads=[("wb", i), hkeys[tb][kk]], writes=[("ps", b)])
                    if tb % 2 == 0:
                        k.copy(ob[o][:, tb * 512:(tb + 1) * 512], ps, reads=[("ps", b)], writes=[("ob", o, tb)], eng="act")
                    else:
                        k.copy(ob[o][:, tb * 512:(tb + 1) * 512], ps, reads=[("ps", b)], writes=[("ob", o, tb)], eng="dve")
                    if is_k and tb < 2:
                        k.copy(okf[o][:, tb * 512:(tb + 1) * 512], ps, reads=[("ps", b)], writes=[("okf", o, tb)],
                               eng="dve" if tb % 2 == 0 else "act")
                k.dma(pT_d[r0 + ch * 128:r0 + (ch + 1) * 128, :], ob[o],
                      reads=[("ob", o, tb) for tb in range(T // 512)], writes=[("pT_d", r0 // 128 + ch)])
                tick(k)
                if is_k and not os.environ.get("DBG_NODMA"):
                    k.dma(nkT[ch * 128:(ch + 1) * 128, :], okf[o], reads=[("okf", o, 0), ("okf", o, 1)],
                          writes=[("nkT", ch)])
    vb = [A.alloc([512], BF16) for _ in range(2)]
    vf = [A.alloc([512]) for _ in range(2)]
    nt = 0
    for cb in range(0 if os.environ.get("DBG_NOV") else 2):
        i = n % NB
        n += 1
        k.dma(wb[i], wv[:, :, 2048 + cb * 512:2048 + (cb + 1) * 512], writes=[("wb", i)], q="pool")
        for ti in range(T // 128):
            b = nps % 4
            nps += 1
            ps = k.ps[b]
            o = nt % 2
            nt += 1
            for kk in range(16):
                k.mm(ps, hT[:, kk, ti * 128:(ti + 1) * 128], wb[i][:, kk, :], kk == 0, kk == 15,
                     reads=[("wb", i), hkeys[ti // 4][kk]], writes=[("ps", b)])
            k.copy(vb[o], ps, reads=[("ps", b)], writes=[("vb", o)], eng="act")
            k.dma(v_tok[ti * 128:(ti + 1) * 128, cb * 512:(cb + 1) * 512], vb[o], reads=[("vb", o)],
                  writes=[("v_tok", ti, cb)])
            if ti < 8:
                k.copy(vf[o], ps, reads=[("ps", b)], writes=[("vf", o)], eng="dve")
                k.dma(nv[ti * 128:(ti + 1) * 128, cb * 512:(cb + 1) * 512], vf[o], reads=[("vf", o)],
                      writes=[("nv", ti, cb)])
    while getattr(k, "bg", None) is not None:
        tick(k)
    k.s.barrier()
    A.off = mark


def transpose_out(k, a_tok, ntile, dst_rows, col0, psb, tagbase):
    A = k.arena
    P = k.P
    mixT_d = k.scratch("mixT_d", [2048, 2048], BF16)
    aT = A.alloc([8, ntile * 128], BF16)
    cnt = 0
    for c in range(8):
        for t0 in range(0, ntile, 4):
            b = psb[cnt % len(psb)]
            cnt += 1
            ps = k.ps[b]
            nn = min(4, ntile - t0)
            for t in range(nn):
                k.mm(ps[:, t * 128:(t + 1) * 128], a_tok[:, t0 + t, c * 128:(c + 1) * 128], P["ident"], True, True,
                     reads=tagbase(c) + ["ident"], writes=[("ps", b)], tr=True)
            k.copy(aT[:, c, t0 * 128:(t0 + nn) * 128], ps[:, 0:nn * 128], reads=[("ps", b)], writes=[("aT", c)],
                   eng="act" if cnt % 2 else "dve")
    k.dma(mixT_d[dst_rows:dst_rows + 1024, col0:col0 + ntile * 128].rearrange("(c p) t -> p c t", p=128), aT,
          reads=[("aT", c) for c in range(8)], writes=[("mixT_d", dst_rows, col0)])


def phase_attn_prompt(k, T):
    A = k.arena
    mark = A.off
    pT_d = k.scratch("pT_d", [5120, T], BF16)
    v_tok = k.scratch("v_tok", [T, 1024], BF16)
    qv = pT_d[0:1024, :].rearrange("(c p) t -> p c t", p=128)
    kv = pT_d[1024:2048, :].rearrange("(c p) t -> p c t", p=128)
    qT = [A.alloc([8, 256], BF16) for _ in range(2)]
    kT = [A.alloc([8, 256], BF16) for _ in range(2)]
    Va = [A.alloc([2, 16, 65], BF16) for _ in range(2)]
    pT = [A.alloc([1024], BF16) for _ in range(2)]
    rc = [A.alloc([4]) for _ in range(2)]
    a_tok = A.alloc([2, 1024])
    for i in range(2):
        k.memset(Va[i][:, :, :, 64:65], 1.0, writes=[("Va1", i)])
    jobs = []

    def make_job(s, hp):
        i = s % 2
        sb = 2 * (hp % 2)
        ob_ = 4 + hp % 2
        pi = hp % 2

        def s_fn():
            if hp == 0:
                cs = slice(s * 256, (s + 1) * 256)
                k.dma(qT[i], qv[:, :, cs], writes=[("qT", i)])
                k.dma(kT[i], kv[:, :, cs], writes=[("kT", i)])
                for t_ in range(2):
                    k.dma(Va[i][:, t_, :, 0:64],
                          v_tok[s * 256 + t_ * 128:s * 256 + (t_ + 1) * 128, :].rearrange("p (h d) -> p h d", h=16),
                          writes=[("Va", i, t_)])
            pss2 = k.psall[:, sb * 512:(sb + 2) * 512]
            for h2 in range(2):
                base = h2 * 64
                for kc in range(2):
                    k.mm(k.ps[sb + h2][:, kc * 256:(kc + 1) * 256], kT[i][base:base + 64, hp, kc * 128:(kc + 1) * 128],
                         qT[i][base:base + 64, hp, :], True, True, reads=[("qT", i), ("kT", i)], writes=[("ps", sb + h2)])
            k.act(pT[pi], pss2, AF.Exp, scale=0.125, reads=[("ps", sb), ("ps", sb + 1)], writes=[("pT", pi)])

        def pv_fn():
            pso = k.ps[ob_]
            for h2 in range(2):
                h = 2 * hp + h2
                for qc in range(2):
                    s_ = h2 * 2 + qc
                    for kc in range(2):
                        k.mm(pso[:, s_ * 128:s_ * 128 + 65],
                             pT[pi][:, h2 * 512 + kc * 256 + qc * 128:h2 * 512 + kc * 256 + (qc + 1) * 128],
                             Va[i][:, kc, h, :], kc == 0, kc == 1, reads=[("pT", pi), ("Va", i, kc), ("Va1", i)],
                             writes=[("ps", ob_)])
            po3 = pso.rearrange("p (s c) -> p s c", s=4)
            k.recip(rc[pi], po3[:, :, 64], reads=[("ps", ob_)], writes=[("rc", pi)])
            k.tt(a_tok[:, :, hp * 128:(hp + 1) * 128].rearrange("p q (h d) -> p h q d", h=2),
                 po3[:, :, 0:64].rearrange("p (h q) d -> p h q d", h=2),
                 rc[pi].rearrange("p (h q) -> p h q", h=2).unsqueeze(3).to_broadcast([128, 2, 2, 64]), ALU.mult,
                 reads=[("ps", ob_), ("rc", pi)], writes=[("a_tok", 2 * hp), ("a_tok", 2 * hp + 1)])
            if hp == 7:
                m2 = A.off
                transpose_out(k, a_tok, 2, 0, s * 256, [6, 7], lambda c: [("a_tok", 2 * c), ("a_tok", 2 * c + 1)])
                A.off = m2

        return s_fn, pv_fn

    for s in range(4):
        for hp in range(8):
            jobs.append(make_job(s, hp))
    jobs[0][0]()
    for n_, (_, pv_fn) in enumerate(jobs):
        if n_ + 1 < len(jobs):
            jobs[n_ + 1][0]()
        pv_fn()
    k.s.barrier()
    A.off = mark


def phase_attn_sample(k, T):
    A = k.arena
    mark = A.off
    rowok, chunks = na_geometry()
    pT_d = k.scratch("pT_d", [5120, T], BF16)
    v_tok = k.scratch("v_tok", [T, 1024], BF16)
    kcT_in = k.inp("kcT", [1024, PAST])
    cv_in = k.inp("cv", [PAST, 1024])
    natt = k.inp("natt", [128, NH, 14, 64])
    S0 = 1024
    qT = A.alloc([8, 1024], BF16)
    kT = A.alloc([8, 1024], BF16)
    kcT = A.alloc([8, PAST], BF16)
    Va = A.alloc([8, 16, 65], BF16)
    Vc = A.alloc([4, 16, 65], BF16)
    ET = A.alloc([NH, 14, 64], BF16)
    a_tok = A.alloc([8, 1024])
    rc = [A.alloc([8]) for _ in range(2)]
    PL = [A.alloc([1024], BF16) for _ in range(3)]
    k.dma(qT, pT_d[0:1024, S0:S0 + 1024].rearrange("(c p) t -> p c t", p=128), writes=["qT"])
    k.dma(kT, pT_d[1024:2048, S0:S0 + 1024].rearrange("(c p) t -> p c t", p=128), writes=["kT"])
    k.dma(kcT, kcT_in.rearrange("(c p) t -> p c t", p=128), writes=["kcT"], q="pool")
    k.memset(Va[:, :, :, 64:65], 1.0, writes=["Va1"])
    k.memset(Vc[:, :, :, 64:65], 1.0, writes=["Vc1"])
    for t_ in range(8):
        k.dma(Va[:, t_, :, 0:64], v_tok[S0 + t_ * 128:S0 + (t_ + 1) * 128, :].rearrange("p (h d) -> p h d", h=16),
              writes=[("Va", t_)])
    for t_ in range(4):
        k.dma(Vc[:, t_, :, 0:64], cv_in[t_ * 128:(t_ + 1) * 128, :].rearrange("p (h d) -> p h d", h=16),
              writes=[("Vc", t_)], q="pool")
    m1 = A.off
    tmpE = A.alloc([14 * 64])
    for h in range(NH):
        k.dma(tmpE, natt[:, h].rearrange("p j q -> p (j q)"), writes=["tmpE"])
        k.act(ET[:, h].rearrange("p j q -> p (j q)"), tmpE, AF.Exp, reads=["tmpE"], writes=["ET"])
    A.off = m1
    jobs = []
    state = {"npl": 0}

    def make_job(h, ci):
        hp, base = h // 2, (h % 2) * 64
        ob0 = 4 + 2 * (h % 2)
        local = ci < 8
        c = ci if local else ci - 8
        sb = 2 * (ci % 2)
        pli = (h * 12 + ci) % 3
        pl = PL[pli]
        plk = ("PL", pli)

        def oacc(i):
            bb = ob0 + i // 4
            return bb, k.ps[bb][:, (i % 4) * 128:(i % 4) * 128 + 65]

        def s_fn():
            if local:
                qlo, qhi = chunks[c]
                nq = qhi - qlo + 1
                ncol = nq * 64
                for p0 in range(0, ncol, 512):
                    pn = min(512, ncol - p0)
                    bb = sb + p0 // 512
                    k.mm(k.ps[bb][:, 0:pn], kT[base:base + 64, hp, c * 128:(c + 1) * 128],
                         qT[base:base + 64, hp, qlo * 64 + p0:qlo * 64 + p0 + pn], True, True,
                         reads=["qT", "kT"], writes=[("ps", bb)])
                    k.act(pl[:, p0:p0 + pn], k.ps[bb][:, 0:pn], AF.Exp, scale=0.125, reads=[("ps", bb)], writes=[plk])
                js = [qr - 2 * c + 6 for qr in range(qlo, qhi + 1)]
                valid = [(0 <= j <= 13) for j in js]
                v0 = valid.index(True)
                v1 = len(valid) - 1 - valid[::-1].index(True)
                pl3 = pl[:, 0:ncol].rearrange("p (r q) -> p r q", q=64)
                k.tt(pl3[:, v0:v1 + 1, :], pl3[:, v0:v1 + 1, :], ET[:, h, js[v0]:js[v1] + 1, :], ALU.mult,
                     reads=[plk, "ET"], writes=[plk])
                for r, qr in enumerate(range(qlo, qhi + 1)):
                    for krl in range(2):
                        if (not valid[r]) or (not rowok[2 * c + krl, qr]):
                            k.memset(pl[krl * 64:(krl + 1) * 64, r * 64:(r + 1) * 64], 0.0, writes=[plk], eng="pool")
            else:
                for p0 in (0, 512):
                    bb = sb + p0 // 512
                    k.mm(k.ps[bb], kcT[base:base + 64, hp, c * 128:(c + 1) * 128], qT[base:base + 64, hp, p0:p0 + 512],
                         True, True, reads=["qT", "kcT"], writes=[("ps", bb)])
                    k.act(pl[:, p0:p0 + 512], k.ps[bb], AF.Exp, scale=0.125, reads=[("ps", bb)], writes=[plk])

        def pv_fn():
            if local:
                qlo, qhi = chunks[c]
                for i in range(qlo // 2, qhi // 2 + 1):
                    bb, oa = oacc(i)
                    first = (h, bb) not in state
                    state[(h, bb)] = True
                    k.mm(oa, pl[:, (2 * i - qlo) * 64:(2 * i - qlo) * 64 + 128], Va[:, c, h, :], first, False,
                         reads=[plk, ("Va", c), "Va1"], writes=[("ps", bb)], sgc=True)
            else:
                for i in range(8):
                    bb, oa = oacc(i)
                    k.mm(oa, pl[:, i * 128:(i + 1) * 128], Vc[:, c, h, :], False, c == 3,
                         reads=[plk, ("Vc", c), "Vc1"], writes=[("ps", bb)], sgc=True)
            if ci == 11:
                for half in range(2):
                    bb = ob0 + half
                    po3 = k.ps[bb].rearrange("p (q c) -> p q c", q=4)
                    k.recip(rc[h % 2][:, half * 4:(half + 1) * 4], po3[:, :, 64], reads=[("ps", bb)],
                            writes=[("rc", h % 2, half)])
                    k.tt(a_tok[:, half * 4:(half + 1) * 4, h * 64:(h + 1) * 64], po3[:, :, 0:64],
                         rc[h % 2][:, half * 4:(half + 1) * 4].unsqueeze(2).to_broadcast([128, 4, 64]), ALU.mult,
                         reads=[("ps", bb), ("rc", h % 2, half)], writes=[("a_tok", h)])

        return s_fn, pv_fn

    for h in range(NH):
        for ci in range(12):
            jobs.append(make_job(h, ci))
    jobs[0][0]()
    for n_, (_, pv_fn) in enumerate(jobs):
        if n_ + 1 < len(jobs):
            jobs[n_ + 1][0]()
        pv_fn()
    transpose_out(k, a_tok, 8, 0, S0, [0, 1, 2, 3], lambda c: [("a_tok", 2 * c), ("a_tok", 2 * c + 1)])
    k.s.barrier()
    A.off = mark


def hyena_consts(L):
    t = np.linspace(0.0, 1.0, L, dtype=np.float32)[:, None]
    omega = (np.float32(2.0 * math.pi) * np.arange(L, dtype=np.float32)[:, None] / np.float32(L)).astype(np.float32)
    bands = np.linspace(1e-4, 15, 16, dtype=np.float32)[None, :]
    z = np.concatenate([t, np.cos(bands * omega), -np.sin(bands * omega)], axis=-1).astype(np.float32)
    max_decay = math.log(1e-2) / 0.3
    min_decay = math.log(1e-2) / 1.5
    deltas = np.abs(np.linspace(min_decay, max_decay, HYW, dtype=np.float32))
    window = (np.exp(-t * deltas[None, :]) + np.float32(0.05)).astype(np.float32)
    tt = np.arange(L, dtype=np.float64)[:, None]
    f = np.arange(L, dtype=np.float64)[None, :]
    Fc = np.cos(math.pi * f * tt / L)
    Fs = -np.sin(math.pi * f * tt / L)
    Fs[:, 0] = np.cos(math.pi * tt[:, 0])
    F = np.concatenate([Fc, Fs], axis=1)
    return dict(zT=np.ascontiguousarray(z.T), window=window,
                F=F.astype(ml_dtypes.bfloat16), FT=np.ascontiguousarray(F.T).astype(ml_dtypes.bfloat16))


def phase_hyena(k, L, seq_cols, T, tag):
    A = k.arena
    P = k.P
    mark = A.off
    NT = L // 128
    NF = 2 * NT
    pT_d = k.scratch("pT_d", [5120, T], BF16)
    mixT_d = k.scratch("mixT_d", [2048, 2048], BF16)
    zT_in = k.inp(f"hy_zT{L}", [33, L])
    win_in = k.inp(f"hy_win{L}", [L, HYW])
    F_in = k.inp(f"hy_F{L}", [L, 2 * L], BF16)
    FT_in = k.inp(f"hy_FT{L}", [2 * L, L], BF16)
    w1_in = k.inp("hy_w1", [33, 64])
    w2_in = k.inp("hy_w2", [64, 64])
    w3_in = k.inp("hy_w3", [64, 2 * HYW])
    fv_in = k.inp("hy_fvec", [64, 3])
    sw_in = k.inp("hy_swT", [128, 24, 3])
    sb_in = k.inp("hy_sbT", [128, 24])
    db_in = k.inp("hy_dbT", [128, 8])
    F = A.alloc([NT, 2 * L], BF16)
    KA = A.alloc([NT, HYW], BF16)
    KB = A.alloc([NT, HYW], BF16)
    sw = A.alloc([24, 3])
    sbv = A.alloc([24])
    db = A.alloc([8])
    k.dma(F, F_in.rearrange("(n p) f -> p n f", p=128), writes=["F"])
    k.dma(sw, sw_in, writes=["sw"])
    k.dma(sbv, sb_in, writes=["sbv"])
    k.dma(db, db_in, writes=["db"])
    m1 = A.off
    a_t = A.alloc([NT, HYW], BF16)
    d_t = A.alloc([NT, HYW], BF16)
    zT = A.alloc([L])
    w1 = A.alloc([64])
    w2 = A.alloc([64])
    w3 = A.alloc([2 * HYW])
    fv = A.alloc([3])
    cst = A.alloc([2])
    h1 = A.alloc([L])
    h2 = A.alloc([L])
    arg = A.alloc([512])
    wrp = A.alloc([512])
    wr2 = A.alloc([512])
    win = [A.alloc([HYW]) for _ in range(2)]
    rsn = A.alloc([HYW])
    tf = [A.alloc([512]) for _ in range(2)]
    tb_ = [A.alloc([512]) for _ in range(2)]
    sq = [A.alloc([512]) for _ in range(2)]
    k.dma(zT[0:33, :], zT_in, writes=["zT"])
    k.dma(w1[0:33, :], w1_in, writes=["w1"])
    k.dma(w2[0:64, :], w2_in, writes=["w2"])
    k.dma(w3[0:64, :], w3_in, writes=["w3"])
    k.dma(fv[0:64, :], fv_in, writes=["fv"])
    k.memset(cst[:, 0:1], -math.pi, writes=["cst"])
    TWO_PI = 2.0 * math.pi

    def sin_layer(dst, lhsT, rhs_src, nk, bcol):
        for p0 in range(0, L, 512):
            pn = min(512, L - p0)
            b = (p0 // 512) % 2
            ps = k.ps[b]
            k.mm(ps[0:64, 0:pn], lhsT, rhs_src[0:nk, p0:p0 + pn], True, True, reads=["zT", "w1", "w2", "h1"],
                 writes=[("ps", b)])
            k.ts(arg[0:64, 0:pn], ps[0:64, 0:pn], fv[0:64, bcol:bcol + 1], ALU.add, reads=[("ps", b), "fv"], writes=["arg"],
                 s2=fv[0:64, 1:2], op1=ALU.mult)
            a_ = arg[0:64, 0:pn]
            w_ = wrp[0:64, 0:pn]
            k.ts(w_, a_, math.pi, ALU.is_gt, reads=["arg"], writes=["wrp"], s2=-TWO_PI, op1=ALU.mult)
            k.stt(w_, a_, -math.pi, w_, ALU.is_lt, ALU.add, reads=["arg", "wrp"], writes=["wrp"]) if False else None
            k.ts(wr2[0:64, 0:pn], a_, -math.pi, ALU.is_lt, reads=["arg"], writes=["wr2"], s2=TWO_PI, op1=ALU.mult)
            k.tt(a_, a_, w_, ALU.add, reads=["arg", "wrp"], writes=["arg"])
            k.tt(a_, a_, wr2[0:64, 0:pn], ALU.add, reads=["arg", "wr2"], writes=["arg"])
            k.act(dst[0:64, p0:p0 + pn], arg[0:64, 0:pn], AF.Sin, reads=["arg"],
                  writes=["h1" if dst is h1 else "h2"])

    sin_layer(h1, w1[0:33, :], zT, 33, 0)
    sin_layer(h2, w2[0:64, :], h1, 64, 2)
    nps = 0
    nq_ = 0
    for ti in range(NT):
        wi = ti % 2
        k.dma(win[wi], win_in[ti * 128:(ti + 1) * 128, :], writes=[("win", wi)])
        for cbk in range(2):
            cs = slice(cbk * 512, (cbk + 1) * 512)
            i_ = (ti * 2 + cbk) % 2
            bf_ = 2 + nps % 4
            nps += 1
            bb_ = 2 + nps % 4
            nps += 1
            k.mm(k.ps[bf_], h2[0:64, ti * 128:(ti + 1) * 128], w3[0:64, cbk * 512:(cbk + 1) * 512], True, True,
                 reads=["h2", "w3"], writes=[("ps", bf_)])
            k.mm(k.ps[bb_], h2[0:64, ti * 128:(ti + 1) * 128], w3[0:64, HYW + cbk * 512:HYW + (cbk + 1) * 512], True, True,
                 reads=["h2", "w3"], writes=[("ps", bb_)])
            k.tt(tf[i_], k.ps[bf_], win[wi][:, cs], ALU.mult, reads=[("ps", bf_), ("win", wi)], writes=[("tf", i_)])
            k.tt(tb_[i_], k.ps[bb_], win[wi][:, cs], ALU.mult, reads=[("ps", bb_), ("win", wi)], writes=[("tb_", i_)])
            if ti == 0:
                k.memset(tb_[i_][0:1, :], 0.0, writes=[("tb_", i_)], eng="dve")
            for src, skey in ((tf[i_], ("tf", i_)), (tb_[i_], ("tb_", i_))):
                s_ = sq[nq_ % 2]
                k.act(s_, src, AF.Square, reads=[skey], writes=[("sq", nq_ % 2)])
                k.mm(k.ps[6 + cbk], P["ones_f"], s_, ti == 0 and src is tf[i_], ti == NT - 1 and src is tb_[i_],
                     reads=[("sq", nq_ % 2), "ones_f"], writes=[("ps", 6 + cbk)])
                nq_ += 1
            k.tt(a_t[:, ti, cs], tf[i_], tb_[i_], ALU.add, reads=[("tf", i_), ("tb_", i_)], writes=[("a_t", ti)], eng="pool")
            k.tt(d_t[:, ti, cs], tf[i_], tb_[i_], ALU.subtract, reads=[("tf", i_), ("tb_", i_)], writes=[("d_t", ti)], eng="pool")
    for cbk in range(2):
        k.act(rsn[:, cbk * 512:(cbk + 1) * 512], k.ps[6 + cbk], AF.Sqrt, bias=P["eps"], reads=[("ps", 6 + cbk), "eps"],
              writes=["rsn"])
    k.recip(rsn, rsn, reads=["rsn"], writes=["rsn"])
    k.ts(rsn, rsn, 1.0 / L, ALU.mult, reads=["rsn"], writes=["rsn"])
    akeys = [("a_t", ti) for ti in range(NT)]
    dkeys = [("d_t", ti) for ti in range(NT)]
    nps = 0
    for fc in range(NT):
        for cbk in range(2):
            cs = slice(cbk * 512, (cbk + 1) * 512)
            b = nps % 4
            nps += 1
            ps = k.ps[b]
            for ti in range(NT):
                k.mm(ps, F[:, ti, fc * 128:(fc + 1) * 128], a_t[:, ti, cs], ti == 0, ti == NT - 1,
                     reads=["F"] + akeys, writes=[("ps", b)])
            k.tt(KA[:, fc, cs], ps, rsn[:, cs], ALU.mult, reads=[("ps", b), "rsn"], writes=[("KA", fc)])
            b = nps % 4
            nps += 1
            ps = k.ps[b]
            for ti in range(NT):
                k.mm(ps, F[:, ti, L + fc * 128:L + (fc + 1) * 128], d_t[:, ti, cs], ti == 0, ti == NT - 1,
                     reads=["F"] + dkeys, writes=[("ps", b)])
            k.tt(KB[:, fc, cs], ps, rsn[:, cs], ALU.mult, reads=[("ps", b), "rsn"], writes=[("KB", fc)])
    k.ts(KA[0:1, 0, :], KA[0:1, 0, :], 0.5, ALU.mult, reads=[("KA", 0)], writes=[("KA", 0)])
    for cbk in range(2):
        cs = slice(cbk * 512, (cbk + 1) * 512)
        b = 4 + cbk
        ps = k.ps[b]
        for ti in range(NT):
            k.mm(ps[0:1, :], F[:, ti, L:L + 1], a_t[:, ti, cs], ti == 0, ti == NT - 1, reads=["F"] + akeys,
                 writes=[("ps", b)])
        k.stt(KB[0:1, 0, cs], ps[0:1, :], 0.5, rsn[0:1, cs], ALU.mult, ALU.mult, reads=[("ps", b), ("KB", 0), "rsn"],
              writes=[("KB", 0)])
    k.s.barrier()
    A.off = m1
    FT = A.alloc([NF, L], BF16)
    k.dma(FT, FT_in.rearrange("(n p) t -> p n t", p=128), writes=["FT"])
    CH = 512
    NCC = CH // 128
    NBUF = 2 if L == 256 else 1
    zb = [A.alloc([3, L], BF16) for _ in range(2)]
    zcb = [A.alloc([3, L]) for _ in range(NBUF)]
    u_allb = [A.alloc([NCC, L]) for _ in range(NBUF)]
    x0_allb = [A.alloc([NCC, L]) for _ in range(NBUF)]
    u_tokb = [A.alloc([NT, CH], BF16) for _ in range(NBUF)]
    Yb = [A.alloc([NF, CH], BF16) for _ in range(NBUF)]
    tqb = [[A.alloc([512]) for _ in range(4)] for _ in range(NBUF)]
    ep = [A.alloc([512]) for _ in range(2)]
    bo = [A.alloc([L], BF16) for _ in range(2)]
    zview = pT_d[2048:5120, :].rearrange("(g c p) t -> p g c t", g=3, c=8)
    cnt = {"nz": 0, "nps": 0, "nbo": 0}

    def make_unit(ui, t0, half):
        bi = ui % NBUF
        u_all, x0_all, u_tok, Y = u_allb[bi], x0_allb[bi], u_tokb[bi], Yb[bi]
        cs = slice(half * CH, (half + 1) * CH)
        utk = [("u_tok", bi, cl) for cl in range(NCC)]
        ykeys = [("Y", bi, f) for f in range(NF)]

        def stage_a():
            for cl in range(NCC):
                cc = half * NCC + cl
                zi = cnt["nz"] % 2
                zq = cnt["nz"] % NBUF
                zc = zcb[zq]
                cnt["nz"] += 1
                k.dma(zb[zi], zview[:, :, cc, t0:t0 + L], writes=[("zb", zi)])
                for g in range(3):
                    wcol = g * 8 + cc
                    k.act(zc[:, g, :], zb[zi][:, g, :], AF.Identity, bias=sbv[:, wcol:wcol + 1], scale=sw[:, wcol, 1:2],
                          reads=[("zb", zi), "sw", "sbv"], writes=[("zc", zq, g)])
                    k.stt(zc[:, g, 1:L], zb[zi][:, g, 0:L - 1], sw[:, wcol, 0:1], zc[:, g, 1:L], ALU.mult, ALU.add,
                          reads=[("zb", zi), "sw", ("zc", zq, g)], writes=[("zc", zq, g)])
                    k.stt(zc[:, g, 0:L - 1], zb[zi][:, g, 1:L], sw[:, wcol, 2:3], zc[:, g, 0:L - 1], ALU.mult, ALU.add,
                          reads=[("zb", zi), "sw", ("zc", zq, g)], writes=[("zc", zq, g)])
                k.tt(u_all[:, cl, :], zc[:, 0, :], zc[:, 1, :], ALU.mult, reads=[("zc", zq, 0), ("zc", zq, 1)],
                     writes=[("u", bi, cl)])
                k.copy(x0_all[:, cl, :], zc[:, 2, :], reads=[("zc", zq, 2)], writes=[("x0", bi, cl)], eng="pool")
                for t4 in range(0, NT, 4):
                    b = cnt["nps"] % 2
                    cnt["nps"] += 1
                    ps = k.ps[b]
                    nn = min(4, NT - t4)
                    for ti in range(nn):
                        k.mm(ps[:, ti * 128:(ti + 1) * 128], u_all[:, cl, (t4 + ti) * 128:(t4 + ti + 1) * 128], P["ident"],
                             True, True, reads=[("u", bi, cl), "ident"], writes=[("ps", b)], tr=True)
                    k.copy(u_tok[:, t4:t4 + nn, cl * 128:(cl + 1) * 128],
                           ps[:, 0:nn * 128].rearrange("p (t c) -> p t c", c=128), reads=[("ps", b)],
                           writes=[("u_tok", bi, cl)], eng="act")

        def stage_b():
            for fa in range(NT):
                ba, bb_ = 2, 3
                if fa % 2:
                    ba, bb_ = 4, 5
                psa = k.ps[ba]
                psb = k.ps[bb_]
                for ti in range(NT):
                    k.mm(psa, F[:, ti, fa * 128:(fa + 1) * 128], u_tok[:, ti, :], ti == 0, ti == NT - 1,
                         reads=["F"] + utk, writes=[("ps", ba)])
                for ti in range(NT):
                    k.mm(psb, F[:, ti, L + fa * 128:L + (fa + 1) * 128], u_tok[:, ti, :], ti == 0, ti == NT - 1,
                         reads=["F"] + utk, writes=[("ps", bb_)])
                tp_ = fa % NBUF
                tq = tqb[tp_]
                k.tt(tq[0], psa, KA[:, fa, cs], ALU.mult, reads=[("ps", ba), ("KA", fa)], writes=[("tq", tp_, 0)])
                k.tt(tq[1], psb, KB[:, fa, cs], ALU.mult, reads=[("ps", bb_), ("KB", fa)], writes=[("tq", tp_, 1)])
                k.tt(tq[2], psa, KB[:, fa, cs], ALU.mult, reads=[("ps", ba), ("KB", fa)], writes=[("tq", tp_, 2)])
                k.tt(tq[3], psb, KA[:, fa, cs], ALU.mult, reads=[("ps", bb_), ("KA", fa)], writes=[("tq", tp_, 3)])
                k.tt(Y[:, fa, :], tq[0], tq[1], ALU.subtract, reads=[("tq", tp_, 0), ("tq", tp_, 1)], writes=[("Y", bi, fa)],
                     eng="pool")
                k.tt(Y[:, NT + fa, :], tq[2], tq[3], ALU.add, reads=[("tq", tp_, 2), ("tq", tp_, 3)],
                     writes=[("Y", bi, NT + fa)], eng="pool")
                if fa == 0:
                    k.copy(Y[0:1, 0, :], tq[0][0:1, :], reads=[("tq", tp_, 0), ("Y", bi, 0)], writes=[("Y", bi, 0)], eng="pool")
                    k.copy(Y[0:1, NT, :], tq[1][0:1, :], reads=[("tq", tp_, 1), ("Y", bi, NT)], writes=[("Y", bi, NT)],
                           eng="pool")

        def stage_c():
            for cl in range(NCC):
                cc = half * NCC + cl
                oi = cnt["nbo"] % 2
                cnt["nbo"] += 1
                for p0 in range(0, L, 512):
                    pn = min(512, L - p0)
                    b = 6 + (cnt["nps"] % 2)
                    cnt["nps"] += 1
                    ps = k.ps[b]
                    for f in range(NF):
                        k.mm(ps[:, 0:pn], Y[:, f, cl * 128:(cl + 1) * 128], FT[:, f, p0:p0 + pn], f == 0, f == NF - 1,
                             reads=ykeys + ["FT"], writes=[("ps", b)])
                    e_ = ep[(p0 // 512) % 2]
                    k.stt(e_[:, 0:pn], u_all[:, cl, p0:p0 + pn], db[:, cc:cc + 1], ps[:, 0:pn], ALU.mult, ALU.add,
                          reads=[("u", bi, cl), "db", ("ps", b)], writes=[("ep", (p0 // 512) % 2)])
                    k.tt(bo[oi][:, p0:p0 + pn], e_[:, 0:pn], x0_all[:, cl, p0:p0 + pn], ALU.mult,
                         reads=[("ep", (p0 // 512) % 2), ("x0", bi, cl)], writes=[("bo", oi)], eng="pool")
                k.dma(mixT_d[1024 + cc * 128:1024 + (cc + 1) * 128, t0:t0 + L], bo[oi], reads=[("bo", oi)],
                      writes=[("mixT_d", "hy", cc, t0)])

        return stage_a, stage_b, stage_c

    units = []
    for t0 in seq_cols:
        for half in range(HYW // CH):
            units.append(make_unit(len(units), t0, half))
    if NBUF == 2:
        units[0][0]()
        for n_, (_, sb_fn, sc_fn) in enumerate(units):
            if n_ + 1 < len(units):
                units[n_ + 1][0]()
            sb_fn()
            sc_fn()
    else:
        for (sa_fn, sb_fn, sc_fn) in units:
            sa_fn()
            sb_fn()
            sc_fn()
    k.s.barrier()
    A.off = mark


_CONST_CACHE = {}


def prep_core_inputs_l0(inp, core, d):
    b = core // 4
    d["w_in_even"] = inp["w_in_even"]
    d["kcT"] = np.ascontiguousarray(inp["cache_k"][b, 0].reshape(PAST, 1024).T)
    d["cv"] = np.ascontiguousarray(inp["cache_v"][b, 0].reshape(PAST, 1024))
    d["natt"] = natt_table(inp["na_rpb"][0])
    for L in (256, 1024):
        if L not in _CONST_CACHE:
            _CONST_CACHE[L] = hyena_consts(L)
        c = _CONST_CACHE[L]
        d[f"hy_zT{L}"] = c["zT"]
        d[f"hy_win{L}"] = c["window"]
        d[f"hy_F{L}"] = c["F"]
        d[f"hy_FT{L}"] = c["FT"]
    d["hy_w1"] = inp["hy_filt_w1"][0]
    d["hy_w2"] = inp["hy_filt_w2"][0]
    d["hy_w3"] = inp["hy_filt_w3"][0]
    d["hy_fvec"] = np.ascontiguousarray(np.stack([inp["hy_filt_b1"][0], inp["hy_filt_freq"][0], inp["hy_filt_b2"][0]], axis=1))
    sw = inp["hy_short_w"][0]
    d["hy_swT"] = np.ascontiguousarray(np.transpose(sw.reshape(3, 24, 128), (2, 1, 0)))
    d["hy_sbT"] = pk(inp["hy_short_b"][0])
    d["hy_dbT"] = pk(inp["hy_bias_d"][0])
    return d


def phase_outproj(k, w_dram, src_x, dst_x, blocks, l):
    A = k.arena
    mark = A.off
    mixT_d = k.scratch("mixT_d", [2048, 2048], BF16)
    wv = w_dram.rearrange("(k p) c -> p k c", p=128)
    W = A.alloc([16, 2048], BF16)
    for cb in range(4):
        k.dma(W[:, :, cb * 512:(cb + 1) * 512], wv[:, :, cb * 512:(cb + 1) * 512], writes=[("W", cb)], q="pool")
    mv = mixT_d.rearrange("(k p) t -> p k t", p=128)
    xv = src_x.rearrange("(k p) t -> p k t", p=128)
    ov = dst_x.rearrange("(k p) t -> p k t", p=128)
    m_rows = mixT_d.rearrange("d (b t) -> (d b) t", t=256)
    x_rows = src_x.rearrange("d (b t) -> (d b) t", t=256)
    mb = [A.alloc([16, 512], BF16) for _ in range(2)]
    xb = [A.alloc([16, 512]) for _ in range(2)]
    nps = 0
    for bi, (src0, n, j, dst0) in enumerate(blocks):
        i = bi % 2
        if src0 is None:
            k.s.rec("pool", None, writes=[("mb", i), ("xb", i)])
            for kk in range(16):
                k.gather(mb[i][:, kk, 0:n], m_rows, k.P["own_idx"][:, kk:kk + 1], reads=["own_idx", ("mb", i)],
                         writes=[("mbg", i, kk)])
                k.gather(xb[i][:, kk, 0:n], x_rows, k.P["own_idx"][:, kk:kk + 1], reads=["own_idx", ("xb", i)],
                         writes=[("xbg", i, kk)])
        else:
            k.dma(mb[i][:, :, 0:n], mv[:, :, src0:src0 + n], writes=[("mb", i)])
            k.dma(xb[i][:, :, 0:n], xv[:, :, src0:src0 + n], writes=[("xb", i)])
        _, _, G_ = mod_views(k, l, 0, j)
        for dch in range(16):
            b = nps % 4
            nps += 1
            ps = k.ps[b]
            for kk in range(16):
                k.mm(ps[:, 0:n], W[:, kk, dch * 128:(dch + 1) * 128], mb[i][:, kk, 0:n], kk == 0, kk == 15,
                     reads=[("W", dch // 4), ("mb", i), ("mbg", i, kk)], writes=[("ps", b)])
            k.stt(xb[i][:, dch, 0:n], ps[:, 0:n], G_[:, dch:dch + 1], xb[i][:, dch, 0:n], ALU.mult, ALU.add,
                  reads=[("ps", b), ("xb", i), ("xbg", i, dch), ("modT", l, j)], writes=[("xb", i)])
        k.dma(ov[:, :, dst0:dst0 + n], xb[i][:, :, 0:n], reads=[("xb", i)], writes=[("xout", bi)], q="pool")
    k.s.barrier()
    A.off = mark


def bc(ap, shape, axis):
    return ap.unsqueeze(axis).to_broadcast(list(shape))


def phase_moe(k, src_x, dst_x, groups, l):
    A = k.arena
    P = k.P
    w1_d = k.inp("moe_w1", [2, NEXP, D, FF])
    w3_d = k.inp("moe_w3", [2, NEXP, D, FF])
    w2_d = k.inp("moe_w2", [2, NEXP, FF, D])
    wr_d = k.inp("moe_wrT", [128, 2, 16, 20])
    br_d = k.inp("moe_brB", [128, 2, 20])
    sel_d = k.inp("moe_sel", [16, 16, 128])
    xv = src_x.rearrange("(k p) t -> p k t", p=128)
    ov = dst_x.rearrange("(k p) t -> p k t", p=128)
    for g, (c0, TG, j) in enumerate(groups):
        mark = A.off
        halves = [(o, min(512, TG - o)) for o in range(0, TG, 512)]
        jh = [j] * len(halves) if isinstance(j, int) else list(j)
        NTI = TG // 128
        xacc = A.alloc([16, TG])
        hT = A.alloc([16, TG], BF16)
        combT = A.alloc([TG])
        sel = A.alloc([16, 128])
        k.dma(sel[0:16], sel_d, writes=["sel"])
        for hb, (o, n) in enumerate(halves):
            k.dma(xacc[:, :, o:o + n], xv[:, :, c0 + o:c0 + o + n], writes=[("xacc", hb)])
        m1 = A.off
        sq = A.alloc([16, 512], BF16)
        rs = A.alloc([512])
        tmp = [A.alloc([512]) for _ in range(2)]
        hf = A.alloc([16, 512])
        wr = A.alloc([16, 20])
        br = A.alloc([20])
        lg = A.alloc([NTI, 20])
        k.dma(wr, wr_d[:, l], writes=["wr"])
        k.dma(br, br_d[:, l], writes=["br"])
        psl = k.ps[7]
        for hb, (o, n) in enumerate(halves):
            hcs = slice(o, o + n)
            j = jh[hb]
            A_, B_, G_ = mod_views(k, l, 1, j)
            k.act(sq[:, :, 0:n], xacc[:, :, hcs], AF.Square, reads=[("xacc", hb)], writes=["sq"])
            ps = k.ps[hb]
            for kk in range(16):
                k.mm(ps[:, 0:n], P["ones_bf"], sq[:, kk, 0:n], kk == 0, kk == 15, reads=["sq", "ones"], writes=[("ps", hb)])
            k.act(rs[:, 0:n], ps[:, 0:n], AF.Sqrt, bias=P["eps"], scale=1.0 / D, reads=[("ps", hb), "eps"], writes=["rs"])
            k.recip(rs[:, 0:n], rs[:, 0:n], reads=["rs"], writes=["rs"])
            k.tt(hf[:, :, 0:n], xacc[:, :, hcs], rs[:, 0:n].unsqueeze(1).to_broadcast([128, 16, n]), ALU.mult,
                 reads=[("xacc", hb), "rs"], writes=["hf"] + [("hfk", kk) for kk in range(16)])
            for kk in range(16):
                k.act(hf[:, kk, 0:n], hf[:, kk, 0:n], AF.Identity, bias=B_[:, kk:kk + 1], scale=A_[:, kk:kk + 1],
                      reads=["hf", ("modT", l, j), ("Amod", l, 1, j)], writes=[("hfk", kk)])
            k.copy(hT[:, :, hcs], hf[:, :, 0:n], reads=["hf"] + [("hfk", kk) for kk in range(16)], writes=[("hT", hb)],
                   eng="dve")
            for t4 in range(n // 128):
                ti = o // 128 + t4
                for kk in range(16):
                    k.mm(psl[:, ti * 20:(ti + 1) * 20], hf[:, kk, t4 * 128:(t4 + 1) * 128], wr[:, kk, :], kk == 0, kk == 15,
                         reads=["hf", ("hfk", kk), "wr"], writes=[("ps", 7)])
        r_ = {}
        for nm, shp in (("gmax", [NTI]), ("ge", [NTI, 4]), ("gs", [NTI]), ("gp", [NTI]), ("goh", [NTI, 4]),
                        ("t44", [NTI, 4, 4]), ("esel", [NTI, 4]), ("m1", [NTI]), ("oh1", [NTI, 4]), ("e2", [NTI, 4]),
                        ("m2", [NTI]), ("oh2", [NTI, 4]), ("dd", [NTI]), ("w1", [NTI]), ("w2", [NTI]), ("ws", [NTI, 4]),
                        ("ws2", [NTI, 4]), ("comb", [NTI, 4, 4])):
            r_[nm] = A.alloc(shp)
        RK = "route"
        k.tt(lg, psl[:, 0:NTI * 20].rearrange("p (t e) -> p t e", e=20), bc(br, [128, NTI, 20], 1), ALU.add,
             reads=[("ps", 7), "br"], writes=[RK])

        def dv(fn):
            k.s.rec("dve", fn, reads=[RK], writes=[RK])

        def route(r_, lg, NTI):
            gl = lg[:, :, 0:4]
            el = lg[:, :, 4:20].rearrange("p t (g j) -> p t g j", j=4)
            s3 = [128, NTI, 4]
            s4 = [128, NTI, 4, 4]
            dv(lambda e: e.tensor_reduce(out=r_["gmax"], in_=gl, axis=AX.X, op=ALU.max))
            dv(lambda e: e.tensor_tensor(out=r_["ge"], in0=gl, in1=bc(r_["gmax"], s3, 2), op=ALU.subtract))
            k.s.rec("act", lambda e: e.activation(out=r_["ge"], in_=r_["ge"], func=AF.Exp), reads=[RK], writes=[RK])
            dv(lambda e: e.tensor_reduce(out=r_["gs"], in_=r_["ge"], axis=AX.X, op=ALU.add))
            dv(lambda e: e.reciprocal(out=r_["gp"], in_=r_["gs"]))
            dv(lambda e: e.tensor_tensor(out=r_["goh"], in0=gl, in1=bc(r_["gmax"], s3, 2), op=ALU.is_ge))
            dv(lambda e: e.tensor_tensor(out=r_["t44"], in0=el, in1=bc(r_["goh"], s4, 3), op=ALU.mult))
            dv(lambda e: e.tensor_reduce(out=r_["esel"], in_=r_["t44"].rearrange("p t g j -> p t j g"), axis=AX.X, op=ALU.add))
            dv(lambda e: e.tensor_reduce(out=r_["m1"], in_=r_["esel"], axis=AX.X, op=ALU.max))
            dv(lambda e: e.tensor_tensor(out=r_["oh1"], in0=r_["esel"], in1=bc(r_["m1"], s3, 2), op=ALU.is_ge))
            dv(lambda e: e.scalar_tensor_tensor(out=r_["e2"], in0=r_["oh1"], scalar=-1e30, in1=r_["esel"], op0=ALU.mult, op1=ALU.add))
            dv(lambda e: e.tensor_reduce(out=r_["m2"], in_=r_["e2"], axis=AX.X, op=ALU.max))
            dv(lambda e: e.tensor_tensor(out=r_["oh2"], in0=r_["e2"], in1=bc(r_["m2"], s3, 2), op=ALU.is_ge))
            dv(lambda e: e.tensor_tensor(out=r_["dd"], in0=r_["m2"], in1=r_["m1"], op=ALU.subtract))
            k.s.rec("act", lambda e: e.activation(out=r_["dd"], in_=r_["dd"], func=AF.Exp), reads=[RK], writes=[RK])
            dv(lambda e: e.tensor_scalar(out=r_["dd"], in0=r_["dd"], scalar1=1.0, scalar2=None, op0=ALU.add))
            dv(lambda e: e.reciprocal(out=r_["w1"], in_=r_["dd"]))
            dv(lambda e: e.tensor_tensor(out=r_["w1"], in0=r_["w1"], in1=r_["gp"], op=ALU.mult))
            dv(lambda e: e.tensor_tensor(out=r_["w2"], in0=r_["gp"], in1=r_["w1"], op=ALU.subtract))
            dv(lambda e: e.tensor_tensor(out=r_["ws"], in0=r_["oh1"], in1=bc(r_["w1"], s3, 2), op=ALU.mult))
            dv(lambda e: e.tensor_tensor(out=r_["ws2"], in0=r_["oh2"], in1=bc(r_["w2"], s3, 2), op=ALU.mult))
            dv(lambda e: e.tensor_tensor(out=r_["ws"], in0=r_["ws"], in1=r_["ws2"], op=ALU.add))
            dv(lambda e: e.tensor_tensor(out=r_["comb"], in0=bc(r_["goh"], s4, 3), in1=bc(r_["ws"], s4, 2), op=ALU.mult))

        route(r_, lg, NTI)
        comb = r_["comb"].rearrange("p t g j -> p t (g j)")
        for hb, (o, n) in enumerate(halves):
            ps = k.ps[hb]
            for t4 in range(n // 128):
                k.mm(ps[0:16, t4 * 128:(t4 + 1) * 128], comb[:, o // 128 + t4, :], P["ident"], True, True,
                     reads=[RK, "ident"], writes=[("ps", hb)], tr=True)
            k.copy(combT[0:16, o:o + n], ps[0:16, 0:n], reads=[("ps", hb)], writes=["combT"])
        k.s.barrier()
        A.off = m1
        NQ = 6
        wq = [A.alloc([16, 128], BF16) for _ in range(NQ)]
        w2q = [A.alloc([4, 512], BF16) for _ in range(4)]
        hid1 = A.alloc([4, TG], BF16)
        hid = [hid1, hid1]
        cb1 = A.alloc([TG])
        cb = [cb1, cb1]
        sl = [A.alloc([512]) for _ in range(2)]
        t3 = [A.alloc([512]) for _ in range(2)]
        nq = 0
        nst = 0
        nps = 0
        for e in range(NEXP):
            ei = e % 2
            w1v = w1_d[l, e].rearrange("(k p) f -> p k f", p=128)
            w3v = w3_d[l, e].rearrange("(k p) f -> p k f", p=128)
            ei = 0
            w2v = w2_d[l, e].rearrange("(c p) d -> p c d", p=128)
            for hb, (o, n) in enumerate(halves):
                b = 6 + hb % 2
                k.mm(k.ps[b][:, 0:n], sel[0:16, e, :], combT[0:16, o:o + n], True, True, reads=["sel", "combT"],
                     writes=[("ps", b)])
                k.copy(cb[ei][:, o:o + n], k.ps[b][:, 0:n], reads=[("ps", b)], writes=[("cb", ei, hb)], eng="act")
            for fq in range(4):
                q1 = nq % NQ
                q3 = (nq + 1) % NQ
                nq += 2
                k.dma(wq[q1], w1v[:, :, fq * 128:(fq + 1) * 128], writes=[("wq", q1)], q="pool")
                k.dma(wq[q3], w3v[:, :, fq * 128:(fq + 1) * 128], writes=[("wq", q3)], q="pool")
                for hb, (o, n) in enumerate(halves):
                    hcs = slice(o, o + n)
                    b1 = (nps % 2)
                    b3 = 2 + (nps % 2)
                    nps += 1
                    for kk in range(16):
                        k.mm(k.ps[b1][:, 0:n], wq[q1][:, kk, :], hT[:, kk, hcs], kk == 0, kk == 15,
                             reads=[("wq", q1), ("hT", hb)], writes=[("ps", b1)])
                    for kk in range(16):
                        k.mm(k.ps[b3][:, 0:n], wq[q3][:, kk, :], hT[:, kk, hcs], kk == 0, kk == 15,
                             reads=[("wq", q3), ("hT", hb)], writes=[("ps", b3)])
                    si = nst % 2
                    nst += 1
                    k.act(sl[si][:, 0:n], k.ps[b1][:, 0:n], AF.Silu, reads=[("ps", b1)], writes=[("sl", si)])
                    k.tt(t3[si][:, 0:n], k.ps[b3][:, 0:n], cb[ei][:, hcs], ALU.mult, reads=[("ps", b3), ("cb", ei, hb)],
                         writes=[("t3", si)])
                    k.tt(hid[ei][:, fq, hcs], sl[si][:, 0:n], t3[si][:, 0:n], ALU.mult, reads=[("sl", si), ("t3", si)],
                         writes=[("hid", ei, hb)], eng="dve")
            for dch in range(16):
                dq = dch // 4
                if dch % 4 == 0:
                    k.dma(w2q[dq], w2v[:, :, dq * 512:(dq + 1) * 512], writes=[("w2q", dq)], q="pool")
                for hb, (o, n) in enumerate(halves):
                    hcs = slice(o, o + n)
                    j = jh[hb]
                    A_, B_, G_ = mod_views(k, l, 1, j)
                    b = 4 + (nps % 2)
                    nps += 1
                    for fq in range(4):
                        k.mm(k.ps[b][:, 0:n], w2q[dq][:, fq, (dch % 4) * 128:(dch % 4 + 1) * 128], hid[ei][:, fq, hcs],
                             fq == 0, fq == 3, reads=[("w2q", dq), ("hid", ei, hb)], writes=[("ps", b)])
                    k.stt(xacc[:, dch, hcs], k.ps[b][:, 0:n], G_[:, dch:dch + 1], xacc[:, dch, hcs], ALU.mult, ALU.add,
                          reads=[("ps", b), ("xacc", hb), ("modT", l, j)], writes=[("xacc", hb)])
        for hb, (o, n) in enumerate(halves):
            k.dma(ov[:, :, c0 + o:c0 + o + n], xacc[:, :, o:o + n], reads=[("xacc", hb)], writes=[("xout", g, hb)])
        k.s.barrier()
        A.off = mark


def odd_consts():
    c = np.arange(256, dtype=np.float64)
    ang = 2.0 * math.pi * np.outer(c, c) / 256.0
    CS = np.concatenate([np.cos(ang), np.sin(ang)], axis=1)
    d = {"od_CS": CS.astype(ml_dtypes.bfloat16)}
    for L in (256, 1024):
        l_ = np.arange(L, dtype=np.float64)
        a = 2.0 * math.pi * np.outer(l_, l_) / L
        d[f"od_CL{L}"] = np.stack([np.cos(a), -np.sin(a)], axis=1).astype(ml_dtypes.bfloat16)
        pos = np.arange(L)
        inv = np.zeros((4, L), np.float32)
        for g, w in enumerate((2, 4, 8, 16)):
            lo = np.clip(pos - w // 2, 0, L)
            hi = np.clip(pos + w // 2, 0, L)
            inv[g] = 1.0 / (hi - lo).astype(np.float32)
        d[f"od_inv{L}"] = np.ascontiguousarray(np.broadcast_to(inv[None], (128, 4, L)))
    return d


def phase_odd(k, src_x, T, cond_of_tb):
    A = k.arena
    mark = A.off
    w_in = k.inp("w_in_odd", [1, D, D])
    wv = w_in[0].rearrange("(k p) c -> p k c", p=128)
    top = 16 * T // 2
    A.words -= top
    pT = A.h[:, A.words:A.words + top].bitcast(BF16).rearrange("p (a b) -> p a b", a=16)
    m0 = A.off
    hT = A.alloc([16, T], BF16)
    phase_prenorm(k, src_x, T, 1, 0, cond_of_tb, hT, "hT", nbuf=2, bw=256)
    hkeys = [[("hT", tb, kk) for kk in range(16)] for tb in range(T // 512)]
    wb = [A.alloc([16, 512], BF16) for _ in range(2)]
    nps = 0
    for cb in range(4):
        i = cb % 2
        k.dma(wb[i], wv[:, :, cb * 512:(cb + 1) * 512], writes=[("wb", i)], q="pool")
        for fc in range(4):
            ch = cb * 4 + fc
            for tb in range(T // 512):
                b = nps % 4
                nps += 1
                for kk in range(16):
                    k.mm(k.ps[b], wb[i][:, kk, fc * 128:(fc + 1) * 128], hT[:, kk, tb * 512:(tb + 1) * 512], kk == 0, kk == 15,
                         reads=[("wb", i), hkeys[tb][kk]], writes=[("ps", b)])
                k.copy(pT[:, ch, tb * 512:(tb + 1) * 512], k.ps[b], reads=[("ps", b)], writes=[("pT", ch)],
                       eng="act" if nps % 2 else "dve")
    k.s.barrier()
    A.off = m0
    phase_odd2(k, pT, T, mark)
    A.words += top


def phase_odd2(k, pT, T, mark):
    A = k.arena
    mixT_d = k.scratch("mixT_d", [2048, 2048], BF16)
    fn_d = k.inp("fn_lin", [1, 4, 256, 256])
    pl_d = k.inp("pool_lin", [1, 4, 256, 256])
    psc_d = k.inp("od_pscT", [128, 8])
    CS_d = k.inp("od_CS", [256, 512], BF16)
    CS = A.alloc([2, 512], BF16)
    k.dma(CS, CS_d.rearrange("(c p) f -> p c f", p=128), writes=["CS"])
    lin = A.alloc([2, 4, 2, 256], BF16)
    for g_ in range(4):
        k.dma(lin[:, 0, g_], fn_d[0, g_].rearrange("(c p) d -> p c d", p=128), writes=[("lin0", g_)], q="pool")
        k.dma(lin[:, 1, g_], pl_d[0, g_].rearrange("(c p) d -> p c d", p=128), writes=[("lin1", g_)], q="pool")
    psc = A.alloc([8])
    k.dma(psc, psc_d, writes=["psc"])
    seqs = [(256, s * 256) for s in range(4)] + [(1024, 1024)]
    cur_L = None
    mL = A.off
    nps = 0
    for (L, t0) in seqs:
        NT = L // 128
        if L != cur_L:
            k.s.barrier()
            A.off = mL
            cur_L = L
            CL_d = k.inp(f"od_CL{L}", [L, 2, L], BF16)
            inv_d = k.inp(f"od_inv{L}", [128, 4, L])
            CL = A.alloc([NT, 2, L], BF16)
            k.dma(CL, CL_d.rearrange("(n p) s l -> p n s l", p=128), writes=["CL"])
            inv = A.alloc([4, L])
            k.dma(inv, inv_d, writes=["inv"])
            ucs = A.alloc([NT, 512], BF16)
            fT = A.alloc([8, L], BF16)
            pm = A.alloc([8, L], BF16)
            pad = [A.alloc([L + 16]) for _ in range(2)]
            sa = [A.alloc([L + 16]) for _ in range(2)]
            sb_ = [A.alloc([L + 16]) for _ in range(2)]
            ob = [A.alloc([L], BF16) for _ in range(2)]
            for i in range(2):
                k.memset(pad[i], 0.0, writes=[("pad", i)])
        scale = 1.0 / math.sqrt(256.0 * L)
        for g in range(4):
            for ti in range(NT):
                b = nps % 4
                nps += 1
                for ch in range(2):
                    k.mm(k.ps[b], pT[:, g * 2 + ch, t0 + ti * 128:t0 + (ti + 1) * 128], CS[:, ch, :], ch == 0, ch == 1,
                         reads=[("pT", g * 2 + ch), "CS"], writes=[("ps", b)])
                k.copy(ucs[:, ti, :], k.ps[b], reads=[("ps", b)], writes=[("ucs", ti)], eng="act" if nps % 2 else "dve")
            ukeys = [("ucs", ti) for ti in range(NT)]
            for cc in range(2):
                for p0 in range(0, L, 512):
                    pn = min(512, L - p0)
                    b = nps % 4
                    nps += 1
                    n = 0
                    for s_ in range(2):
                        for ti in range(NT):
                            k.mm(k.ps[b][:, 0:pn], ucs[:, ti, s_ * 256 + cc * 128:s_ * 256 + (cc + 1) * 128],
                                 CL[:, ti, s_, p0:p0 + pn], n == 0, n == 2 * NT - 1, reads=ukeys + ["CL"], writes=[("ps", b)])
                            n += 1
                    k.act(fT[:, g * 2 + cc, p0:p0 + pn], k.ps[b][:, 0:pn], AF.Copy, scale=scale, reads=[("ps", b)],
                          writes=[("fT", g * 2 + cc)])
        for c8 in range(8):
            g = c8 // 2
            w = (2, 4, 8, 16)[g]
            i = c8 % 2
            e1 = "pool" if c8 % 2 else "dve"
            k.copy(pad[i][:, 8:8 + L], pT[:, 8 + c8, t0:t0 + L], reads=[("pT", 8 + c8)], writes=[("pad", i)], eng=e1)
            W_ = L + 16
            k.tt(sa[i][:, 0:W_ - 1], pad[i][:, 0:W_ - 1], pad[i][:, 1:W_], ALU.add, reads=[("pad", i)], writes=[("sa", i)], eng=e1)
            cur, curk, oth, othk, span = sa[i], ("sa", i), sb_[i], ("sb", i), 2
            while span < w:
                k.tt(oth[:, 0:W_ - 2 * span + 1], cur[:, 0:W_ - 2 * span + 1], cur[:, span:W_ - span + 1], ALU.add,
                     reads=[curk], writes=[othk], eng=e1)
                cur, curk, oth, othk = oth, othk, cur, curk
                span *= 2
            o0 = 8 - w // 2
            k.tt(oth[:, 0:L], cur[:, o0:o0 + L], inv[:, g, :], ALU.mult, reads=[curk, "inv"], writes=[othk], eng=e1)
            k.tt(pm[:, c8, :], oth[:, 0:L], pad[i][:, 8:8 + L], ALU.subtract, reads=[othk, ("pad", i)], writes=[("pm", c8)], eng=e1)
        no = 0
        for which, src, skey in ((0, fT, "fT"), (1, pm, "pm")):
            for g in range(4):
                for dc in range(2):
                    oi = no % 2
                    no += 1
                    for p0 in range(0, L, 512):
                        pn = min(512, L - p0)
                        b = nps % 4
                        nps += 1
                        for cc in range(2):
                            k.mm(k.ps[b][:, 0:pn], lin[:, which, g, cc, dc * 128:(dc + 1) * 128], src[:, g * 2 + cc, p0:p0 + pn],
                                 cc == 0, cc == 1, reads=[(f"lin{which}", g), (skey, g * 2), (skey, g * 2 + 1)], writes=[("ps", b)])
                        if which == 0:
                            k.copy(ob[oi][:, p0:p0 + pn], k.ps[b][:, 0:pn], reads=[("ps", b)], writes=[("ob", oi)], eng="act")
                        else:
                            k.ts(ob[oi][:, p0:p0 + pn], k.ps[b][:, 0:pn], psc[:, g * 2 + dc:g * 2 + dc + 1], ALU.mult,
                                 reads=[("ps", b), "psc"], writes=[("ob", oi)])
                    row = which * 1024 + (g * 2 + dc) * 128
                    k.dma(mixT_d[row:row + 128, t0:t0 + L], ob[oi], reads=[("ob", oi)], writes=[("mixT_d", row, t0)])
    k.s.barrier()
    A.off = mark


def phase_final(k, src_x, T):
    A = k.arena
    P = k.P
    mark = A.off
    yT = k.out("yT", [D, T])
    xv = src_x.rearrange("(k p) t -> p k t", p=128)
    ov = yT.rearrange("(k p) t -> p k t", p=128)
    xb = [A.alloc([16, 512]) for _ in range(2)]
    sq = A.alloc([16, 512], BF16)
    rs = A.alloc([512])
    nf = P["normT"][:, 4, :]
    for tb, o in enumerate(range(0, T, 512)):
        n = min(512, T - o)
        b = tb % 2
        cs = slice(o, o + n)
        k.dma(xb[b][:, :, 0:n], xv[:, :, cs], writes=[("xb", b)])
        k.act(sq[:, :, 0:n], xb[b][:, :, 0:n], AF.Square, reads=[("xb", b)], writes=["sq"])
        ps = k.ps[b]
        for kk in range(16):
            k.mm(ps[:, 0:n], P["ones_bf"], sq[:, kk, 0:n], kk == 0, kk == 15, reads=["sq", "ones"], writes=[("ps", b)])
        k.act(rs[:, 0:n], ps[:, 0:n], AF.Sqrt, bias=P["eps"], scale=1.0 / D, reads=[("ps", b), "eps"], writes=["rs"])
        k.recip(rs[:, 0:n], rs[:, 0:n], reads=["rs"], writes=["rs"])
        k.tt(xb[b][:, :, 0:n], xb[b][:, :, 0:n], rs[:, 0:n].unsqueeze(1).to_broadcast([128, 16, n]), ALU.mult,
             reads=[("xb", b), "rs"], writes=[("xb", b)])
        k.tt(xb[b][:, :, 0:n], xb[b][:, :, 0:n], nf.unsqueeze(2).to_broadcast([128, 16, n]), ALU.mult,
             reads=[("xb", b), "normT"], writes=[("xb", b)])
        k.dma(ov[:, :, cs], xb[b][:, :, 0:n], reads=[("xb", b)], writes=[("yout", tb)], q="pool")
    k.s.barrier()
    A.off = mark


def build_full(dbg=False):
    k = new_builder(dbg)
    T = 2048
    mark = k.arena.off
    phase_mod(k)
    xT = k.inp("xT", [D, T])
    cond = lambda tb: 0 if tb < 2 else 1
    hT = k.arena.alloc([16, T], BF16)
    phase_prenorm(k, xT, T, 0, 0, cond, hT, "hT")
    phase_inproj0(k, hT, T)
    k.arena.off = mark
    phase_attn_prompt(k, T)
    phase_attn_sample(k, T)
    phase_hyena(k, 256, [0, 256, 512, 768], T, "p")
    phase_hyena(k, 1024, [1024], T, "s")
    T1 = 1280
    x1 = k.scratch("x1T", [D, T])
    x2 = k.scratch("x2T", [D, T])
    x3 = k.scratch("x3T", [D, T1])
    x4 = k.scratch("x4T", [D, T1])
    w_oe = k.inp("w_out_even", [1, D, D])
    w_oo = k.inp("w_out_odd", [1, D, D])
    oi = k.inp("own_idx", [128, 16], I32)
    k.dma(k.P["own_idx"], oi, writes=["own_idx"])
    blk0 = [(tb * 512, 512, cond(tb), tb * 512) for tb in range(4)]
    phase_outproj(k, w_oe[0], xT, x1, blk0, 0)
    phase_moe(k, x1, x2, [(0, 1024, 0), (1024, 1024, 1)], 0)
    phase_odd(k, x2, T, cond)
    blk1 = [(0, 512, 0, 0), (512, 512, 0, 512), (None, 256, 1, 1024)]
    phase_outproj(k, w_oo[0], x2, x3, blk1, 1)
    phase_moe(k, x3, x4, [(0, 1280, (0, 0, 1))], 1)
    phase_final(k, x4, T1)
    nc = finish(k)
    return nc, k


_SHARED = {}


def prep_shared(inp):
    d = {}
    for n in ("w_out_even", "w_out_odd", "w_in_odd", "fn_lin", "pool_lin", "moe_w1", "moe_w3", "moe_w2"):
        d[n] = inp[n]
    wr = np.concatenate([inp["moe_w_group"], inp["moe_w_expert"]], axis=-1)
    d["moe_wrT"] = np.ascontiguousarray(np.transpose(wr.reshape(2, 16, 128, 20), (2, 0, 1, 3)))
    br = np.concatenate([inp["moe_b_group"], inp["moe_b_expert"]], axis=-1)
    d["moe_brB"] = np.ascontiguousarray(np.broadcast_to(br[None], (128, 2, 20)))
    sel = np.zeros((16, 16, 128), np.float32)
    for e in range(16):
        sel[e, e, :] = 1.0
    d["moe_sel"] = sel
    if "od" not in _CONST_CACHE:
        _CONST_CACHE["od"] = odd_consts()
    d.update(_CONST_CACHE["od"])
    d["od_pscT"] = pk(inp["pool_scale"][0])
    return d


def kernel(**inputs):
    inp = {k_: np.asarray(v) for k_, v in inputs.items()}
    nc, k = build_full(False)
    shared = prep_shared(inp)
    in_maps = []
    for core in range(NCORES):
        ci = prep_core_inputs(inp, core)
        prep_core_inputs_l0(inp, core, ci)
        ci.update(shared)
        in_maps.append({n: ci[n] for n in k.inputs})
    res = run_bass_kernel_spmd(nc, in_maps, core_ids=list(range(NCORES)))
    y_p = np.zeros((32, SEQ, D), np.float32)
    y_s = np.zeros((2, DSEQ, D), np.float32)
    nk = np.zeros((32, 1, SEQ, NH, HD), np.float32)
    nv = np.zeros((32, 1, SEQ, NH, HD), np.float32)
    for core in range(NCORES):
        r = res.results[core]
        b, q = core // 4, core % 4
        yT = r["yT"]
        y_p[core * 4:(core + 1) * 4] = yT[:, 0:1024].T.reshape(4, SEQ, D)
        y_s[b, q * 256:(q + 1) * 256] = yT[:, 1024:1280].T
        nk[core * 4:(core + 1) * 4, 0] = r["nkT"].T.reshape(4, SEQ, NH, HD)
        nv[core * 4:(core + 1) * 4, 0] = r["nv"].reshape(4, SEQ, NH, HD)
    return y_p, y_s, nk, nv
```

```python
import math
from contextlib import ExitStack

import numpy as np
import ml_dtypes

import concourse.bass as bass
import concourse.mybir as mybir
from concourse.bass_utils import run_bass_kernel_spmd

F32 = mybir.dt.float32
BF16 = mybir.dt.bfloat16
I32 = mybir.dt.int32
AF = mybir.ActivationFunctionType
ALU = mybir.AluOpType
AX = mybir.AxisListType

D = 2048
NCORES = 8
SEQ = 256
DSEQ = 1024
PAST = 512
NH = 16
HD = 64
NAW = 1024
HYW = 1024
GRID_W = 64
EPS = 1e-6
NEXP = 16
FF = 512

ENG = ("pe", "act", "dve", "pool", "sp")


class Op:
    __slots__ = ("eng", "fn", "deps", "needs_inc", "sem", "val", "dma", "slot")

    def __init__(self, eng, fn, dma):
        self.eng = eng
        self.fn = fn
        self.deps = set()
        self.needs_inc = False
        self.sem = None
        self.val = 0
        self.dma = dma
        self.slot = None


class Sched:
    def __init__(self, nc, ring=14):
        self.nc = nc
        self.ops = {e: [] for e in ENG}
        self.last_w = {}
        self.readers = {}
        self.ring = ring
        self.ring_last = {q: [None] * ring for q in ("sp", "pool", "act")}
        self.ring_next = {q: 0 for q in ("sp", "pool", "act")}
        self.ring_cnt = {q: [0] * ring for q in ("sp", "pool", "act")}
        self.live_dma = []
        self.n = 0

    def rec(self, eng, fn, reads=(), writes=(), dma=False, extra=()):
        op = Op(eng, fn, dma)
        deps = set(extra)
        for k in reads:
            w = self.last_w.get(k)
            if w is not None:
                deps.add(w)
            if isinstance(k, tuple) and k[0] == "ps":
                for r in self.readers.get(k, ()):
                    if r.eng != eng:
                        deps.add(r)
        for k in writes:
            w = self.last_w.get(k)
            if w is not None:
                deps.add(w)
            for r in self.readers.get(k, ()):
                deps.add(r)
        if dma:
            q = eng
            i = self.ring_next[q]
            self.ring_next[q] = (i + 1) % self.ring
            prev = self.ring_last[q][i]
            if prev is not None:
                deps.add(prev)
            self.ring_last[q][i] = op
            self.ring_cnt[q][i] += 1
            op.slot = (q, i)
            op.val = 16 * self.ring_cnt[q][i]
            op.needs_inc = True
            self.live_dma.append(op)
        deps.discard(op)
        if eng == "pe" and not dma:
            deps = {d for d in deps if not (d.eng == "pe" and not d.dma)}
        for d in deps:
            d.needs_inc = True
        op.deps = deps
        for k in reads:
            self.readers.setdefault(k, []).append(op)
        for k in writes:
            self.last_w[k] = op
            self.readers[k] = []
        self.ops[eng].append(op)
        self.n += 1
        return op

    def barrier(self):
        lasts = [self.ops[e][-1] for e in ENG if self.ops[e]]
        extra = set(lasts) | set(self.live_dma)
        for e in ENG:
            self.rec(e, None, extra=extra)
        self.live_dma = []
        self.last_w = {}
        self.readers = {}

    def emit(self, es):
        nc = self.nc
        esem = {e: es.enter_context(nc.semaphore("s_" + e)) for e in ENG}
        dsem = {}
        for q in ("sp", "pool", "act"):
            if any(c > 0 for c in self.ring_cnt[q]):
                for i in range(self.ring):
                    if self.ring_cnt[q][i] > 0:
                        dsem[(q, i)] = es.enter_context(nc.semaphore(f"d_{q}{i}"))
        for e in ENG:
            c = 0
            for op in self.ops[e]:
                if op.dma:
                    op.sem = dsem[op.slot]
                elif op.needs_inc and op.fn is not None:
                    c += 1
                    op.sem = esem[e]
                    op.val = c
                else:
                    op.sem = None
        block = es.enter_context(nc.Block())

        def run(e, eng):
            waited = {}
            for op in self.ops[e]:
                for d in op.deps:
                    if d.sem is None:
                        continue
                    key = id(d.sem)
                    if waited.get(key, 0) < d.val:
                        eng.wait_ge(d.sem, d.val)
                        waited[key] = d.val
                if op.fn is None:
                    continue
                inst = op.fn(eng)
                if op.dma:
                    inst.then_inc(op.sem, 16)
                elif op.sem is not None:
                    inst.then_inc(op.sem, 1)

        @block.tensor
        def _(t):
            run("pe", t)

        @block.scalar
        def _(a):
            run("act", a)

        @block.vector
        def _(v):
            run("dve", v)

        @block.gpsimd
        def _(g):
            run("pool", g)

        @block.sync
        def _(s):
            run("sp", s)


class Arena:
    def __init__(self, handle, words):
        self.h = handle
        self.words = words
        self.off = 0

    def alloc(self, free_shape, dtype=F32, parts=128):
        n = int(np.prod(free_shape))
        bpe = 2 if dtype == BF16 else 4
        w = (n * bpe + 3) // 4
        w = (w + 7) // 8 * 8
        assert self.off + w <= self.words, f"arena overflow {self.off}+{w}>{self.words}"
        ap = self.h[:, self.off:self.off + w]
        self.off += w
        if dtype != F32:
            ap = ap.bitcast(dtype)
        ap = ap[:, 0:n]
        if len(free_shape) == 2:
            ap = ap.rearrange("p (a b) -> p a b", a=free_shape[0])
        elif len(free_shape) == 3:
            ap = ap.rearrange("p (a b c) -> p a b c", a=free_shape[0], b=free_shape[1])
        elif len(free_shape) == 4:
            ap = ap.rearrange("p (a b c d) -> p a b c d", a=free_shape[0], b=free_shape[1], c=free_shape[2])
        if parts != 128:
            ap = ap[0:parts]
        return ap


class K:
    def __init__(self, nc, dbg=False):
        self.nc = nc
        self.dbg = dbg
        self.s = Sched(nc)
        self.inputs = {}
        self.outputs = {}
        self.scr = {}
        self.uid = 0

    def inp(self, name, shape, dtype=F32):
        if name not in self.inputs:
            self.inputs[name] = self.nc.dram_tensor(name, list(shape), dtype, kind="ExternalInput").ap()
        return self.inputs[name]

    def out(self, name, shape, dtype=F32):
        if name not in self.outputs:
            self.outputs[name] = self.nc.dram_tensor(name, list(shape), dtype, kind="ExternalOutput").ap()
        return self.outputs[name]

    def scratch(self, name, shape, dtype=F32):
        if name not in self.scr:
            if self.dbg:
                self.scr[name] = self.out(name, shape, dtype)
            else:
                self.scr[name] = self.nc.dram_tensor(name, list(shape), dtype).ap()
        return self.scr[name]

    def dma(self, out, in_, reads=(), writes=(), q="sp"):
        return self.s.rec(q, lambda e: e.dma_start(out=out, in_=in_), reads=reads, writes=writes, dma=True)

    def gather(self, out, in_rows, idx, reads=(), writes=()):
        return self.s.rec("pool", lambda e: e.indirect_dma_start(out=out, out_offset=None, in_=in_rows,
                                                                 in_offset=bass.IndirectOffsetOnAxis(ap=idx, axis=0)),
                          reads=reads, writes=writes, dma=True)

    def mm(self, ps, lhsT, rhs, start, stop, reads, writes, tr=False, sgc=False):
        if tr:
            fn = lambda e: e.matmul(ps, lhsT=lhsT, rhs=rhs, is_transpose=True)
        elif sgc:
            fn = lambda e: e.matmul(ps, lhsT=lhsT, rhs=rhs, start=start, stop=stop, skip_group_check=True)
        else:
            fn = lambda e: e.matmul(ps, lhsT=lhsT, rhs=rhs, start=start, stop=stop)
        return self.s.rec("pe", fn, reads=reads, writes=writes)

    def act(self, out, in_, func, reads, writes, bias=None, scale=None, accum_out=None):
        kw = {}
        if bias is not None:
            kw["bias"] = bias
        if scale is not None:
            kw["scale"] = scale
        if accum_out is not None:
            kw["accum_out"] = accum_out
        return self.s.rec("act", lambda e: e.activation(out=out, in_=in_, func=func, **kw), reads=reads, writes=writes)

    def tt(self, out, in0, in1, op, reads, writes, eng="dve"):
        return self.s.rec(eng, lambda e: e.tensor_tensor(out=out, in0=in0, in1=in1, op=op), reads=reads, writes=writes)

    def ts(self, out, in0, s1, op0, reads, writes, s2=None, op1=None, eng="dve"):
        if op1 is None:
            fn = lambda e: e.tensor_scalar(out=out, in0=in0, scalar1=s1, scalar2=None, op0=op0)
        else:
            fn = lambda e: e.tensor_scalar(out=out, in0=in0, scalar1=s1, scalar2=s2, op0=op0, op1=op1)
        return self.s.rec(eng, fn, reads=reads, writes=writes)

    def stt(self, out, in0, scalar, in1, op0, op1, reads, writes):
        return self.s.rec("dve", lambda e: e.scalar_tensor_tensor(out=out, in0=in0, scalar=scalar, in1=in1, op0=op0, op1=op1),
                          reads=reads, writes=writes)

    def copy(self, out, in_, reads, writes, eng="dve"):
        if eng == "act":
            return self.s.rec("act", lambda e: e.copy(out=out, in_=in_), reads=reads, writes=writes)
        return self.s.rec(eng, lambda e: e.tensor_copy(out=out, in_=in_), reads=reads, writes=writes)

    def memset(self, ap, val, writes, eng="pool"):
        return self.s.rec(eng, lambda e: e.memset(ap, val), writes=writes)

    def recip(self, out, in_, reads, writes):
        return self.s.rec("dve", lambda e: e.reciprocal(out=out, in_=in_), reads=reads, writes=writes)


def setup_persist(k):
    A = k.arena
    P = {}
    P["ones_bf"] = A.alloc([128], BF16)
    P["ones_f"] = A.alloc([128])
    P["ident"] = A.alloc([128])
    P["eps"] = A.alloc([1])
    P["modT"] = A.alloc([2, 2, 96])
    P["normT"] = A.alloc([5, 16])
    P["Amod"] = A.alloc([2, 2, 2, 16])
    k.memset(P["ones_bf"], 1.0, writes=["ones"])
    k.memset(P["ones_f"], 1.0, writes=["ones_f"])
    k.memset(P["eps"], EPS, writes=["eps"])
    k.memset(P["ident"], 0.0, writes=["ident"])
    idn = P["ident"]
    k.s.rec("pool", lambda e: e.affine_select(out=idn, in_=idn, pattern=[[-1, 128]], compare_op=ALU.not_equal,
                                             fill=1.0, base=0, channel_multiplier=1), reads=["ident"], writes=["ident"])
    P["own_idx"] = A.alloc([16], I32)
    k.P = P
    return P


def mod_views(k, l, which, j):
    P = k.P
    m = P["modT"]
    base = which * 3
    A_ = P["Amod"][:, l, which, j, :]
    B_ = m[:, l, j, (base + 0) * 16:(base + 1) * 16]
    G_ = m[:, l, j, (base + 2) * 16:(base + 3) * 16]
    return A_, B_, G_


def mod_evac(k, bT, l, ps3, ch0, ch1, whiches):
    P = k.P
    for j in range(2):
        k.tt(P["modT"][:, l, j, ch0:ch1], ps3[:, ch0:ch1, j], bT[:, l, ch0:ch1], ALU.add, reads=[("modps", l, ch0), "bT"],
             writes=[("modT", l, j)])
        for which in whiches:
            sc = P["modT"][:, l, j, (which * 3 + 1) * 16:(which * 3 + 2) * 16]
            nrm = P["normT"][:, (2 * which + l), :]
            k.stt(P["Amod"][:, l, which, j, :], sc, 1.0, nrm, ALU.add, ALU.mult,
                  reads=[("modT", l, j), "normT"], writes=[("Amod", l, which, j)])


def phase_mod(k):
    A = k.arena
    P = k.P
    ada_w = k.inp("ada_w", [2, D, 6 * D])
    cT = k.inp("cT", [128, 16, 2])
    ada_bT = k.inp("ada_bT", [128, 2, 96])
    normT = k.inp("normT", [128, 5, 16])
    c32 = A.alloc([16, 2])
    sT = A.alloc([16, 2], BF16)
    bT = A.alloc([2, 96])
    k.dma(c32, cT, writes=["c32"])
    k.dma(bT, ada_bT, writes=["bT"])
    k.dma(P["normT"], normT, writes=["normT"])
    k.act(sT, c32, AF.Silu, reads=["c32"], writes=["sT"])
    NB = 3
    wb = [A.alloc([16, 512], BF16) for _ in range(NB)]
    cnt = [0]

    def block(l, cb, ps3, key):
        wv = ada_w[l].rearrange("(k p) c -> p k c", p=128)
        i = cnt[0] % NB
        cnt[0] += 1
        k.dma(wb[i], wv[:, :, cb * 512:(cb + 1) * 512], writes=[("mwb", i)], q="pool")
        for fc in range(4):
            ch = cb * 4 + fc
            for kk in range(16):
                k.mm(ps3[:, ch, :], wb[i][:, kk, fc * 128:(fc + 1) * 128], sT[:, kk, :], kk == 0, kk == 15,
                     reads=[("mwb", i), "sT"], writes=[key])

    def view(b):
        return k.ps[b][:, 0:192].rearrange("p (c j) -> p c j", j=2)

    for cb in range(8):
        block(0, cb, view(4), ("modps", 0, 0))
    mod_evac(k, bT, 0, view(4), 0, 32, [0])

    def gen():
        for cb in range(8, 24):
            block(0, cb, view(6), ("modps", 0, 32))
            yield
        mod_evac(k, bT, 0, view(6), 32, 96, [1])
        for cb in range(24):
            block(1, cb, view(7), ("modps", 1, 0))
            yield
        mod_evac(k, bT, 1, view(7), 0, 96, [0, 1])

    k.bg = gen()


def tick(k):
    if getattr(k, "bg", None) is not None:
        try:
            next(k.bg)
        except StopIteration:
            k.bg = None


def phase_prenorm(k, src, T, l, which, cond_of_tb, hT, hkey, hf_cb=None, nbuf=2, bw=512):
    A = k.arena
    P = k.P
    mark = A.off
    xv = src.rearrange("(k p) t -> p k t", p=128)
    xb = [A.alloc([16, bw]) for _ in range(nbuf)]
    if nbuf == 1:
        xb = [xb[0], xb[0]]
    sq = A.alloc([16, bw], BF16)
    rs = A.alloc([bw])
    hf = None
    nb = T // bw
    for tb in range(nb):
        b = tb % nbuf
        tbk = (tb * bw) // 512
        k.dma(xb[b], xv[:, :, tb * bw:(tb + 1) * bw], writes=[("xb", b)])
        k.act(sq, xb[b], AF.Square, reads=[("xb", b)], writes=["sq"])
        pb = 2 + tb % 2
        ps = k.ps[pb][:, 0:bw]
        for kk in range(16):
            k.mm(ps, P["ones_bf"], sq[:, kk, :], kk == 0, kk == 15, reads=["sq", "ones"], writes=[("ps", pb)])
        k.act(rs, ps, AF.Sqrt, bias=P["eps"], scale=1.0 / D, reads=[("ps", pb), "eps"], writes=["rs"])
        k.recip(rs, rs, reads=["rs"], writes=["rs"])
        j = cond_of_tb(tbk)
        A_, B_, _ = mod_views(k, l, which, j)
        k.tt(xb[b], xb[b], rs.unsqueeze(1).to_broadcast([128, 16, bw]), ALU.mult, reads=[("xb", b), "rs"],
             writes=[("xb", b)])
        for kk in range(16):
            k.act(hT[:, kk, tb * bw:(tb + 1) * bw], xb[b][:, kk, :], AF.Identity, bias=B_[:, kk:kk + 1],
                  scale=A_[:, kk:kk + 1], reads=[("xb", b), ("modT", l, j), ("Amod", l, which, j)], writes=[(hkey, tbk, kk)])
        if hf_cb is not None:
            hf_cb(tb, hf)
    k.s.barrier()
    A.off = mark


ARENA_WORDS = 52992


def new_builder(dbg=False):
    nc = bass.Bass("TRN2", target_bir_lowering=False)
    k = K(nc, dbg)
    es = ExitStack()
    arena_h = es.enter_context(nc.sbuf_tensor("arena", [128, ARENA_WORDS], F32))
    k.arena = Arena(arena_h, ARENA_WORDS)
    k.psall = es.enter_context(nc.psum_tensor("psall", [128, 4096], F32))
    k.ps = [k.psall[:, i * 512:(i + 1) * 512] for i in range(8)]
    k.es = es
    setup_persist(k)
    return k


def finish(k):
    k.s.barrier()
    k.s.emit(k.es)
    k.es.close()
    return k.nc


def pk(v):
    v = np.asarray(v)
    lead = v.shape[:-1]
    n = v.shape[-1] // 128
    r = v.reshape(*lead, n, 128)
    return np.ascontiguousarray(np.moveaxis(r, -1, 0))


def prep_core_inputs(inp, core):
    b = core // 4
    d = {}
    xp = inp["x_prompt"][core * 4:(core + 1) * 4].reshape(4 * SEQ, D)
    xs = inp["x_sample"][b]
    d["xT"] = np.ascontiguousarray(np.concatenate([xp, xs], axis=0).T)
    cv = np.stack([inp["c_ctx"], inp["c"][b]], axis=0)
    d["cT"] = np.ascontiguousarray(np.transpose(cv.reshape(2, 16, 128), (2, 1, 0)))
    d["ada_w"] = inp["ada_w"]
    d["ada_bT"] = pk(inp["ada_b"])
    nv = np.stack([inp["norm_mix"][0], inp["norm_mix"][1], inp["norm_ffn"][0], inp["norm_ffn"][1],
                   inp["norm_final"]], axis=0)
    d["normT"] = pk(nv)
    r = core % 4
    d["own_idx"] = ((np.arange(16)[None, :] * 128 + np.arange(128)[:, None]) * 8 + 4 + r).astype(np.int32)
    return d


def na_geometry():
    rows = DSEQ // GRID_W
    rs = np.clip(np.arange(rows) - 4, 0, rows - 8)
    rowok = np.zeros((rows, rows), bool)
    for qr in range(rows):
        rowok[rs[qr]:rs[qr] + 8, qr] = True
    chunks = []
    for c in range(8):
        ok = rowok[2 * c] | rowok[2 * c + 1]
        q = np.nonzero(ok)[0]
        qlo, qhi = int(q.min()), int(q.max())
        qlo -= qlo % 2
        qhi += 1 - (qhi % 2)
        chunks.append((qlo, qhi))
    return rowok, chunks


def natt_table(rpb):
    kc = np.arange(64)[:, None]
    qc = np.arange(64)[None, :]
    wc0 = np.clip(qc - 8, 0, 48)
    colok = (kc >= wc0) & (kc < wc0 + 16)
    dc = np.clip(kc - qc + 15, 0, 30)
    out = np.full((2, 64, NH, 14, 64), -1e30, np.float32)
    for krl in range(2):
        for j in range(14):
            dr = krl + 13 - j
            g = rpb[:, dr][:, dc]
            g = np.where(colok[None], g, np.float32(-1e30))
            out[krl, :, :, j, :] = np.transpose(g, (1, 0, 2))
    return np.ascontiguousarray(out.reshape(128, NH, 14, 64))


def phase_inproj0(k, hT, T):
    A = k.arena
    mark = A.off
    w = k.inp("w_in_even", [1, D, 6144])
    wv = w[0].rearrange("(k p) c -> p k c", p=128)
    pT_d = k.scratch("pT_d", [5120, T], BF16)
    v_tok = k.scratch("v_tok", [T, 1024], BF16)
    nkT = k.out("nkT", [1024, 1024])
    nv = k.out("nv", [1024, 1024])
    NB = 3
    wb = [A.alloc([16, 512], BF16) for _ in range(NB)]
    ob = [A.alloc([T], BF16) for _ in range(2)]
    okf = [A.alloc([1024]) for _ in range(2)]
    n = 0
    nps = 0
    hkeys = [[("hT", tb, kk) for kk in range(16)] for tb in range(T // 512)]
    groups = [(0, 8, 0), (1024, 8, 1024), (3072, 24, 2048)]
    import os
    if os.environ.get("DBG_GROUPS"):
        groups = [groups[int(c)] for c in os.environ["DBG_GROUPS"] if c.isdigit()]
    nchunk = 0
    for (c0, nch, r0) in groups:
        for cb in range(nch // 4):
            i = n % NB
            n += 1
            k.dma(wb[i], wv[:, :, c0 + cb * 512:c0 + (cb + 1) * 512], writes=[("wb", i)], q="pool")
            for fc in range(4):
                ch = cb * 4 + fc
                o = nchunk % 2
                nchunk += 1
                is_k = (c0 == 1024)
                for tb in range(T // 512):
                    b = nps % 4
                    nps += 1
                    ps = k.ps[b]
                    for kk in range(16):
                        k.mm(ps, wb[i][:, kk, fc * 128:(fc + 1) * 128], hT[:, kk, tb * 512:(tb + 1) * 512],
                             kk == 0, kk == 15, reads=[("wb", i), hkeys[tb][kk]], writes=[("ps", b)])
                    if tb % 2 == 0:
                        k.copy(ob[o][:, tb * 512:(tb + 1) * 512], ps, reads=[("ps", b)], writes=[("ob", o, tb)], eng="act")
                    else:
                        k.copy(ob[o][:, tb * 512:(tb + 1) * 512], ps, reads=[("ps", b)], writes=[("ob", o, tb)], eng="dve")
                    if is_k and tb < 2:
                        k.copy(okf[o][:, tb * 512:(tb + 1) * 512], ps, reads=[("ps", b)], writes=[("okf", o, tb)],
                               eng="dve" if tb % 2 == 0 else "act")
                k.dma(pT_d[r0 + ch * 128:r0 + (ch + 1) * 128, :], ob[o],
                      reads=[("ob", o, tb) for tb in range(T // 512)], writes=[("pT_d", r0 // 128 + ch)])
                tick(k)
                if is_k and not os.environ.get("DBG_NODMA"):
                    k.dma(nkT[ch * 128:(ch + 1) * 128, :], okf[o], reads=[("okf", o, 0), ("okf", o, 1)],
                          writes=[("nkT", ch)])
    vb = [A.alloc([512], BF16) for _ in range(2)]
    vf = [A.alloc([512]) for _ in range(2)]
    nt = 0
    for cb in range(0 if os.environ.get("DBG_NOV") else 2):
        i = n % NB
        n += 1
        k.dma(wb[i], wv[:, :, 2048 + cb * 512:2048 + (cb + 1) * 512], writes=[("wb", i)], q="pool")
        for ti in range(T // 128):
            b = nps % 4
            nps += 1
            ps = k.ps[b]
            o = nt % 2
            nt += 1
            for kk in range(16):
                k.mm(ps, hT[:, kk, ti * 128:(ti + 1) * 128], wb[i][:, kk, :], kk == 0, kk == 15,
                     reads=[("wb", i), hkeys[ti // 4][kk]], writes=[("ps", b)])
            k.copy(vb[o], ps, reads=[("ps", b)], writes=[("vb", o)], eng="act")
            k.dma(v_tok[ti * 128:(ti + 1) * 128, cb * 512:(cb + 1) * 512], vb[o], reads=[("vb", o)],
                  writes=[("v_tok", ti, cb)])
            if ti < 8:
                k.copy(vf[o], ps, reads=[("ps", b)], writes=[("vf", o)], eng="dve")
                k.dma(nv[ti * 128:(ti + 1) * 128, cb * 512:(cb + 1) * 512], vf[o], reads=[("vf", o)],
                      writes=[("nv", ti, cb)])
    while getattr(k, "bg", None) is not None:
        tick(k)
    k.s.barrier()
    A.off = mark


def transpose_out(k, a_tok, ntile, dst_rows, col0, psb, tagbase):
    A = k.arena
    P = k.P
    mixT_d = k.scratch("mixT_d", [2048, 2048], BF16)
    aT = A.alloc([8, ntile * 128], BF16)
    cnt = 0
    for c in range(8):
        for t0 in range(0, ntile, 4):
            b = psb[cnt % len(psb)]
            cnt += 1
            ps = k.ps[b]
            nn = min(4, ntile - t0)
            for t in range(nn):
                k.mm(ps[:, t * 128:(t + 1) * 128], a_tok[:, t0 + t, c * 128:(c + 1) * 128], P["ident"], True, True,
                     reads=tagbase(c) + ["ident"], writes=[("ps", b)], tr=True)
            k.copy(aT[:, c, t0 * 128:(t0 + nn) * 128], ps[:, 0:nn * 128], reads=[("ps", b)], writes=[("aT", c)],
                   eng="act" if cnt % 2 else "dve")
    k.dma(mixT_d[dst_rows:dst_rows + 1024, col0:col0 + ntile * 128].rearrange("(c p) t -> p c t", p=128), aT,
          reads=[("aT", c) for c in range(8)], writes=[("mixT_d", dst_rows, col0)])


def phase_attn_prompt(k, T):
    A = k.arena
    mark = A.off
    pT_d = k.scratch("pT_d", [5120, T], BF16)
    v_tok = k.scratch("v_tok", [T, 1024], BF16)
    qv = pT_d[0:1024, :].rearrange("(c p) t -> p c t", p=128)
    kv = pT_d[1024:2048, :].rearrange("(c p) t -> p c t", p=128)
    qT = [A.alloc([8, 256], BF16) for _ in range(2)]
    kT = [A.alloc([8, 256], BF16) for _ in range(2)]
    Va = [A.alloc([2, 16, 65], BF16) for _ in range(2)]
    pT = [A.alloc([1024], BF16) for _ in range(2)]
    rc = [A.alloc([4]) for _ in range(2)]
    a_tok = A.alloc([2, 1024])
    for i in range(2):
        k.memset(Va[i][:, :, :, 64:65], 1.0, writes=[("Va1", i)])
    jobs = []

    def make_job(s, hp):
        i = s % 2
        sb = 2 * (hp % 2)
        ob_ = 4 + hp % 2
        pi = hp % 2

        def s_fn():
            if hp == 0:
                cs = slice(s * 256, (s + 1) * 256)
                k.dma(qT[i], qv[:, :, cs], writes=[("qT", i)])
                k.dma(kT[i], kv[:, :, cs], writes=[("kT", i)])
                for t_ in range(2):
                    k.dma(Va[i][:, t_, :, 0:64],
                          v_tok[s * 256 + t_ * 128:s * 256 + (t_ + 1) * 128, :].rearrange("p (h d) -> p h d", h=16),
                          writes=[("Va", i, t_)])
            pss2 = k.psall[:, sb * 512:(sb + 2) * 512]
            for h2 in range(2):
                base = h2 * 64
                for kc in range(2):
                    k.mm(k.ps[sb + h2][:, kc * 256:(kc + 1) * 256], kT[i][base:base + 64, hp, kc * 128:(kc + 1) * 128],
                         qT[i][base:base + 64, hp, :], True, True, reads=[("qT", i), ("kT", i)], writes=[("ps", sb + h2)])
            k.act(pT[pi], pss2, AF.Exp, scale=0.125, reads=[("ps", sb), ("ps", sb + 1)], writes=[("pT", pi)])

        def pv_fn():
            pso = k.ps[ob_]
            for h2 in range(2):
                h = 2 * hp + h2
                for qc in range(2):
                    s_ = h2 * 2 + qc
                    for kc in range(2):
                        k.mm(pso[:, s_ * 128:s_ * 128 + 65],
                             pT[pi][:, h2 * 512 + kc * 256 + qc * 128:h2 * 512 + kc * 256 + (qc + 1) * 128],
                             Va[i][:, kc, h, :], kc == 0, kc == 1, reads=[("pT", pi), ("Va", i, kc), ("Va1", i)],
                             writes=[("ps", ob_)])
            po3 = pso.rearrange("p (s c) -> p s c", s=4)
            k.recip(rc[pi], po3[:, :, 64], reads=[("ps", ob_)], writes=[("rc", pi)])
            k.tt(a_tok[:, :, hp * 128:(hp + 1) * 128].rearrange("p q (h d) -> p h q d", h=2),
                 po3[:, :, 0:64].rearrange("p (h q) d -> p h q d", h=2),
                 rc[pi].rearrange("p (h q) -> p h q", h=2).unsqueeze(3).to_broadcast([128, 2, 2, 64]), ALU.mult,
                 reads=[("ps", ob_), ("rc", pi)], writes=[("a_tok", 2 * hp), ("a_tok", 2 * hp + 1)])
            if hp == 7:
                m2 = A.off
                transpose_out(k, a_tok, 2, 0, s * 256, [6, 7], lambda c: [("a_tok", 2 * c), ("a_tok", 2 * c + 1)])
                A.off = m2

        return s_fn, pv_fn

    for s in range(4):
        for hp in range(8):
            jobs.append(make_job(s, hp))
    jobs[0][0]()
    for n_, (_, pv_fn) in enumerate(jobs):
        if n_ + 1 < len(jobs):
            jobs[n_ + 1][0]()
        pv_fn()
    k.s.barrier()
    A.off = mark


def phase_attn_sample(k, T):
    A = k.arena
    mark = A.off
    rowok, chunks = na_geometry()
    pT_d = k.scratch("pT_d", [5120, T], BF16)
    v_tok = k.scratch("v_tok", [T, 1024], BF16)
    kcT_in = k.inp("kcT", [1024, PAST])
    cv_in = k.inp("cv", [PAST, 1024])
    natt = k.inp("natt", [128, NH, 14, 64])
    S0 = 1024
    qT = A.alloc([8, 1024], BF16)
    kT = A.alloc([8, 1024], BF16)
    kcT = A.alloc([8, PAST], BF16)
    Va = A.alloc([8, 16, 65], BF16)
    Vc = A.alloc([4, 16, 65], BF16)
    ET = A.alloc([NH, 14, 64], BF16)
    a_tok = A.alloc([8, 1024])
    rc = [A.alloc([8]) for _ in range(2)]
    PL = [A.alloc([1024], BF16) for _ in range(3)]
    k.dma(qT, pT_d[0:1024, S0:S0 + 1024].rearrange("(c p) t -> p c t", p=128), writes=["qT"])
    k.dma(kT, pT_d[1024:2048, S0:S0 + 1024].rearrange("(c p) t -> p c t", p=128), writes=["kT"])
    k.dma(kcT, kcT_in.rearrange("(c p) t -> p c t", p=128), writes=["kcT"], q="pool")
    k.memset(Va[:, :, :, 64:65], 1.0, writes=["Va1"])
    k.memset(Vc[:, :, :, 64:65], 1.0, writes=["Vc1"])
    for t_ in range(8):
        k.dma(Va[:, t_, :, 0:64], v_tok[S0 + t_ * 128:S0 + (t_ + 1) * 128, :].rearrange("p (h d) -> p h d", h=16),
              writes=[("Va", t_)])
    for t_ in range(4):
        k.dma(Vc[:, t_, :, 0:64], cv_in[t_ * 128:(t_ + 1) * 128, :].rearrange("p (h d) -> p h d", h=16),
              writes=[("Vc", t_)], q="pool")
    m1 = A.off
    tmpE = A.alloc([14 * 64])
    for h in range(NH):
        k.dma(tmpE, natt[:, h].rearrange("p j q -> p (j q)"), writes=["tmpE"])
        k.act(ET[:, h].rearrange("p j q -> p (j q)"), tmpE, AF.Exp, reads=["tmpE"], writes=["ET"])
    A.off = m1
    jobs = []
    state = {"npl": 0}

    def make_job(h, ci):
        hp, base = h // 2, (h % 2) * 64
        ob0 = 4 + 2 * (h % 2)
        local = ci < 8
        c = ci if local else ci - 8
        sb = 2 * (ci % 2)
        pli = (h * 12 + ci) % 3
        pl = PL[pli]
        plk = ("PL", pli)

        def oacc(i):
            bb = ob0 + i // 4
            return bb, k.ps[bb][:, (i % 4) * 128:(i % 4) * 128 + 65]

        def s_fn():
            if local:
                qlo, qhi = chunks[c]
                nq = qhi - qlo + 1
                ncol = nq * 64
                for p0 in range(0, ncol, 512):
                    pn = min(512, ncol - p0)
                    bb = sb + p0 // 512
                    k.mm(k.ps[bb][:, 0:pn], kT[base:base + 64, hp, c * 128:(c + 1) * 128],
                         qT[base:base + 64, hp, qlo * 64 + p0:qlo * 64 + p0 + pn], True, True,
                         reads=["qT", "kT"], writes=[("ps", bb)])
                    k.act(pl[:, p0:p0 + pn], k.ps[bb][:, 0:pn], AF.Exp, scale=0.125, reads=[("ps", bb)], writes=[plk])
                js = [qr - 2 * c + 6 for qr in range(qlo, qhi + 1)]
                valid = [(0 <= j <= 13) for j in js]
                v0 = valid.index(True)
                v1 = len(valid) - 1 - valid[::-1].index(True)
                pl3 = pl[:, 0:ncol].rearrange("p (r q) -> p r q", q=64)
                k.tt(pl3[:, v0:v1 + 1, :], pl3[:, v0:v1 + 1, :], ET[:, h, js[v0]:js[v1] + 1, :], ALU.mult,
                     reads=[plk, "ET"], writes=[plk])
                for r, qr in enumerate(range(qlo, qhi + 1)):
                    for krl in range(2):
                        if (not valid[r]) or (not rowok[2 * c + krl, qr]):
                            k.memset(pl[krl * 64:(krl + 1) * 64, r * 64:(r + 1) * 64], 0.0, writes=[plk], eng="pool")
            else:
                for p0 in (0, 512):
                    bb = sb + p0 // 512
                    k.mm(k.ps[bb], kcT[base:base + 64, hp, c * 128:(c + 1) * 128], qT[base:base + 64, hp, p0:p0 + 512],
                         True, True, reads=["qT", "kcT"], writes=[("ps", bb)])
                    k.act(pl[:, p0:p0 + 512], k.ps[bb], AF.Exp, scale=0.125, reads=[("ps", bb)], writes=[plk])

        def pv_fn():
            if local:
                qlo, qhi = chunks[c]
                for i in range(qlo // 2, qhi // 2 + 1):
                    bb, oa = oacc(i)
                    first = (h, bb) not in state
                    state[(h, bb)] = True
                    k.mm(oa, pl[:, (2 * i - qlo) * 64:(2 * i - qlo) * 64 + 128], Va[:, c, h, :], first, False,
                         reads=[plk, ("Va", c), "Va1"], writes=[("ps", bb)], sgc=True)
            else:
                for i in range(8):
                    bb, oa = oacc(i)
                    k.mm(oa, pl[:, i * 128:(i + 1) * 128], Vc[:, c, h, :], False, c == 3,
                         reads=[plk, ("Vc", c), "Vc1"], writes=[("ps", bb)], sgc=True)
            if ci == 11:
                for half in range(2):
                    bb = ob0 + half
                    po3 = k.ps[bb].rearrange("p (q c) -> p q c", q=4)
                    k.recip(rc[h % 2][:, half * 4:(half + 1) * 4], po3[:, :, 64], reads=[("ps", bb)],
                            writes=[("rc", h % 2, half)])
                    k.tt(a_tok[:, half * 4:(half + 1) * 4, h * 64:(h + 1) * 64], po3[:, :, 0:64],
                         rc[h % 2][:, half * 4:(half + 1) * 4].unsqueeze(2).to_broadcast([128, 4, 64]), ALU.mult,
                         reads=[("ps", bb), ("rc", h % 2, half)], writes=[("a_tok", h)])

        return s_fn, pv_fn

    for h in range(NH):
        for ci in range(12):
            jobs.append(make_job(h, ci))
    jobs[0][0]()
    for n_, (_, pv_fn) in enumerate(jobs):
        if n_ + 1 < len(jobs):
            jobs[n_ + 1][0]()
        pv_fn()
    transpose_out(k, a_tok, 8, 0, S0, [0, 1, 2, 3], lambda c: [("a_tok", 2 * c), ("a_tok", 2 * c + 1)])
    k.s.barrier()
    A.off = mark


def hyena_consts(L):
    t = np.linspace(0.0, 1.0, L, dtype=np.float32)[:, None]
    omega = (np.float32(2.0 * math.pi) * np.arange(L, dtype=np.float32)[:, None] / np.float32(L)).astype(np.float32)
    bands = np.linspace(1e-4, 15, 16, dtype=np.float32)[None, :]
    z = np.concatenate([t, np.cos(bands * omega), -np.sin(bands * omega)], axis=-1).astype(np.float32)
    max_decay = math.log(1e-2) / 0.3
    min_decay = math.log(1e-2) / 1.5
    deltas = np.abs(np.linspace(min_decay, max_decay, HYW, dtype=np.float32))
    window = (np.exp(-t * deltas[None, :]) + np.float32(0.05)).astype(np.float32)
    tt = np.arange(L, dtype=np.float64)[:, None]
    f = np.arange(L, dtype=np.float64)[None, :]
    Fc = np.cos(math.pi * f * tt / L)
    Fs = -np.sin(math.pi * f * tt / L)
    Fs[:, 0] = np.cos(math.pi * tt[:, 0])
    F = np.concatenate([Fc, Fs], axis=1)
    return dict(zT=np.ascontiguousarray(z.T), window=window,
                F=F.astype(ml_dtypes.bfloat16), FT=np.ascontiguousarray(F.T).astype(ml_dtypes.bfloat16))


def phase_hyena(k, L, seq_cols, T, tag):
    A = k.arena
    P = k.P
    mark = A.off
    NT = L // 128
    NF = 2 * NT
    pT_d = k.scratch("pT_d", [5120, T], BF16)
    mixT_d = k.scratch("mixT_d", [2048, 2048], BF16)
    zT_in = k.inp(f"hy_zT{L}", [33, L])
    win_in = k.inp(f"hy_win{L}", [L, HYW])
    F_in = k.inp(f"hy_F{L}", [L, 2 * L], BF16)
    FT_in = k.inp(f"hy_FT{L}", [2 * L, L], BF16)
    w1_in = k.inp("hy_w1", [33, 64])
    w2_in = k.inp("hy_w2", [64, 64])
    w3_in = k.inp("hy_w3", [64, 2 * HYW])
    fv_in = k.inp("hy_fvec", [64, 3])
    sw_in = k.inp("hy_swT", [128, 24, 3])
    sb_in = k.inp("hy_sbT", [128, 24])
    db_in = k.inp("hy_dbT", [128, 8])
    F = A.alloc([NT, 2 * L], BF16)
    KA = A.alloc([NT, HYW], BF16)
    KB = A.alloc([NT, HYW], BF16)
    sw = A.alloc([24, 3])
    sbv = A.alloc([24])
    db = A.alloc([8])
    k.dma(F, F_in.rearrange("(n p) f -> p n f", p=128), writes=["F"])
    k.dma(sw, sw_in, writes=["sw"])
    k.dma(sbv, sb_in, writes=["sbv"])
    k.dma(db, db_in, writes=["db"])
    m1 = A.off
    a_t = A.alloc([NT, HYW], BF16)
    d_t = A.alloc([NT, HYW], BF16)
    zT = A.alloc([L])
    w1 = A.alloc([64])
    w2 = A.alloc([64])
    w3 = A.alloc([2 * HYW])
    fv = A.alloc([3])
    cst = A.alloc([2])
    h1 = A.alloc([L])
    h2 = A.alloc([L])
    arg = A.alloc([512])
    wrp = A.alloc([512])
    wr2 = A.alloc([512])
    win = [A.alloc([HYW]) for _ in range(2)]
    rsn = A.alloc([HYW])
    tf = [A.alloc([512]) for _ in range(2)]
    tb_ = [A.alloc([512]) for _ in range(2)]
    sq = [A.alloc([512]) for _ in range(2)]
    k.dma(zT[0:33, :], zT_in, writes=["zT"])
    k.dma(w1[0:33, :], w1_in, writes=["w1"])
    k.dma(w2[0:64, :], w2_in, writes=["w2"])
    k.dma(w3[0:64, :], w3_in, writes=["w3"])
    k.dma(fv[0:64, :], fv_in, writes=["fv"])
    k.memset(cst[:, 0:1], -math.pi, writes=["cst"])
    TWO_PI = 2.0 * math.pi

    def sin_layer(dst, lhsT, rhs_src, nk, bcol):
        for p0 in range(0, L, 512):
            pn = min(512, L - p0)
            b = (p0 // 512) % 2
            ps = k.ps[b]
            k.mm(ps[0:64, 0:pn], lhsT, rhs_src[0:nk, p0:p0 + pn], True, True, reads=["zT", "w1", "w2", "h1"],
                 writes=[("ps", b)])
            k.ts(arg[0:64, 0:pn], ps[0:64, 0:pn], fv[0:64, bcol:bcol + 1], ALU.add, reads=[("ps", b), "fv"], writes=["arg"],
                 s2=fv[0:64, 1:2], op1=ALU.mult)
            a_ = arg[0:64, 0:pn]
            w_ = wrp[0:64, 0:pn]
            k.ts(w_, a_, math.pi, ALU.is_gt, reads=["arg"], writes=["wrp"], s2=-TWO_PI, op1=ALU.mult)
            k.stt(w_, a_, -math.pi, w_, ALU.is_lt, ALU.add, reads=["arg", "wrp"], writes=["wrp"]) if False else None
            k.ts(wr2[0:64, 0:pn], a_, -math.pi, ALU.is_lt, reads=["arg"], writes=["wr2"], s2=TWO_PI, op1=ALU.mult)
            k.tt(a_, a_, w_, ALU.add, reads=["arg", "wrp"], writes=["arg"])
            k.tt(a_, a_, wr2[0:64, 0:pn], ALU.add, reads=["arg", "wr2"], writes=["arg"])
            k.act(dst[0:64, p0:p0 + pn], arg[0:64, 0:pn], AF.Sin, reads=["arg"],
                  writes=["h1" if dst is h1 else "h2"])

    sin_layer(h1, w1[0:33, :], zT, 33, 0)
    sin_layer(h2, w2[0:64, :], h1, 64, 2)
    nps = 0
    nq_ = 0
    for ti in range(NT):
        wi = ti % 2
        k.dma(win[wi], win_in[ti * 128:(ti + 1) * 128, :], writes=[("win", wi)])
        for cbk in range(2):
            cs = slice(cbk * 512, (cbk + 1) * 512)
            i_ = (ti * 2 + cbk) % 2
            bf_ = 2 + nps % 4
            nps += 1
            bb_ = 2 + nps % 4
            nps += 1
            k.mm(k.ps[bf_], h2[0:64, ti * 128:(ti + 1) * 128], w3[0:64, cbk * 512:(cbk + 1) * 512], True, True,
                 reads=["h2", "w3"], writes=[("ps", bf_)])
            k.mm(k.ps[bb_], h2[0:64, ti * 128:(ti + 1) * 128], w3[0:64, HYW + cbk * 512:HYW + (cbk + 1) * 512], True, True,
                 reads=["h2", "w3"], writes=[("ps", bb_)])
            k.tt(tf[i_], k.ps[bf_], win[wi][:, cs], ALU.mult, reads=[("ps", bf_), ("win", wi)], writes=[("tf", i_)])
            k.tt(tb_[i_], k.ps[bb_], win[wi][:, cs], ALU.mult, reads=[("ps", bb_), ("win", wi)], writes=[("tb_", i_)])
            if ti == 0:
                k.memset(tb_[i_][0:1, :], 0.0, writes=[("tb_", i_)], eng="dve")
            for src, skey in ((tf[i_], ("tf", i_)), (tb_[i_], ("tb_", i_))):
                s_ = sq[nq_ % 2]
                k.act(s_, src, AF.Square, reads=[skey], writes=[("sq", nq_ % 2)])
                k.mm(k.ps[6 + cbk], P["ones_f"], s_, ti == 0 and src is tf[i_], ti == NT - 1 and src is tb_[i_],
                     reads=[("sq", nq_ % 2), "ones_f"], writes=[("ps", 6 + cbk)])
                nq_ += 1
            k.tt(a_t[:, ti, cs], tf[i_], tb_[i_], ALU.add, reads=[("tf", i_), ("tb_", i_)], writes=[("a_t", ti)], eng="pool")
            k.tt(d_t[:, ti, cs], tf[i_], tb_[i_], ALU.subtract, reads=[("tf", i_), ("tb_", i_)], writes=[("d_t", ti)], eng="pool")
    for cbk in range(2):
        k.act(rsn[:, cbk * 512:(cbk + 1) * 512], k.ps[6 + cbk], AF.Sqrt, bias=P["eps"], reads=[("ps", 6 + cbk), "eps"],
              writes=["rsn"])
    k.recip(rsn, rsn, reads=["rsn"], writes=["rsn"])
    k.ts(rsn, rsn, 1.0 / L, ALU.mult, reads=["rsn"], writes=["rsn"])
    akeys = [("a_t", ti) for ti in range(NT)]
    dkeys = [("d_t", ti) for ti in range(NT)]
    nps = 0
    for fc in range(NT):
        for cbk in range(2):
            cs = slice(cbk * 512, (cbk + 1) * 512)
            b = nps % 4
            nps += 1
            ps = k.ps[b]
            for ti in range(NT):
                k.mm(ps, F[:, ti, fc * 128:(fc + 1) * 128], a_t[:, ti, cs], ti == 0, ti == NT - 1,
                     reads=["F"] + akeys, writes=[("ps", b)])
            k.tt(KA[:, fc, cs], ps, rsn[:, cs], ALU.mult, reads=[("ps", b), "rsn"], writes=[("KA", fc)])
            b = nps % 4
            nps += 1
            ps = k.ps[b]
            for ti in range(NT):
                k.mm(ps, F[:, ti, L + fc * 128:L + (fc + 1) * 128], d_t[:, ti, cs], ti == 0, ti == NT - 1,
                     reads=["F"] + dkeys, writes=[("ps", b)])
            k.tt(KB[:, fc, cs], ps, rsn[:, cs], ALU.mult, reads=[("ps", b), "rsn"], writes=[("KB", fc)])
    k.ts(KA[0:1, 0, :], KA[0:1, 0, :], 0.5, ALU.mult, reads=[("KA", 0)], writes=[("KA", 0)])
    for cbk in range(2):
        cs = slice(cbk * 512, (cbk + 1) * 512)
        b = 4 + cbk
        ps = k.ps[b]
        for ti in range(NT):
            k.mm(ps[0:1, :], F[:, ti, L:L + 1], a_t[:, ti, cs], ti == 0, ti == NT - 1, reads=["F"] + akeys,
                 writes=[("ps", b)])
        k.stt(KB[0:1, 0, cs], ps[0:1, :], 0.5, rsn[0:1, cs], ALU.mult, ALU.mult, reads=[("ps", b), ("KB", 0), "rsn"],
              writes=[("KB", 0)])
    k.s.barrier()
    A.off = m1
    FT = A.alloc([NF, L], BF16)
    k.dma(FT, FT_in.rearrange("(n p) t -> p n t", p=128), writes=["FT"])
    CH = 512
    NCC = CH // 128
    NBUF = 2 if L == 256 else 1
    zb = [A.alloc([3, L], BF16) for _ in range(2)]
    zcb = [A.alloc([3, L]) for _ in range(NBUF)]
    u_allb = [A.alloc([NCC, L]) for _ in range(NBUF)]
    x0_allb = [A.alloc([NCC, L]) for _ in range(NBUF)]
    u_tokb = [A.alloc([NT, CH], BF16) for _ in range(NBUF)]
    Yb = [A.alloc([NF, CH], BF16) for _ in range(NBUF)]
    tqb = [[A.alloc([512]) for _ in range(4)] for _ in range(NBUF)]
    ep = [A.alloc([512]) for _ in range(2)]
    bo = [A.alloc([L], BF16) for _ in range(2)]
    zview = pT_d[2048:5120, :].rearrange("(g c p) t -> p g c t", g=3, c=8)
    cnt = {"nz": 0, "nps": 0, "nbo": 0}

    def make_unit(ui, t0, half):
        bi = ui % NBUF
        u_all, x0_all, u_tok, Y = u_allb[bi], x0_allb[bi], u_tokb[bi], Yb[bi]
        cs = slice(half * CH, (half + 1) * CH)
        utk = [("u_tok", bi, cl) for cl in range(NCC)]
        ykeys = [("Y", bi, f) for f in range(NF)]

        def stage_a():
            for cl in range(NCC):
                cc = half * NCC + cl
                zi = cnt["nz"] % 2
                zq = cnt["nz"] % NBUF
                zc = zcb[zq]
                cnt["nz"] += 1
                k.dma(zb[zi], zview[:, :, cc, t0:t0 + L], writes=[("zb", zi)])
                for g in range(3):
                    wcol = g * 8 + cc
                    k.act(zc[:, g, :], zb[zi][:, g, :], AF.Identity, bias=sbv[:, wcol:wcol + 1], scale=sw[:, wcol, 1:2],
                          reads=[("zb", zi), "sw", "sbv"], writes=[("zc", zq, g)])
                    k.stt(zc[:, g, 1:L], zb[zi][:, g, 0:L - 1], sw[:, wcol, 0:1], zc[:, g, 1:L], ALU.mult, ALU.add,
                          reads=[("zb", zi), "sw", ("zc", zq, g)], writes=[("zc", zq, g)])
                    k.stt(zc[:, g, 0:L - 1], zb[zi][:, g, 1:L], sw[:, wcol, 2:3], zc[:, g, 0:L - 1], ALU.mult, ALU.add,
                          reads=[("zb", zi), "sw", ("zc", zq, g)], writes=[("zc", zq, g)])
                k.tt(u_all[:, cl, :], zc[:, 0, :], zc[:, 1, :], ALU.mult, reads=[("zc", zq, 0), ("zc", zq, 1)],
                     writes=[("u", bi, cl)])
                k.copy(x0_all[:, cl, :], zc[:, 2, :], reads=[("zc", zq, 2)], writes=[("x0", bi, cl)], eng="pool")
                for t4 in range(0, NT, 4):
                    b = cnt["nps"] % 2
                    cnt["nps"] += 1
                    ps = k.ps[b]
                    nn = min(4, NT - t4)
                    for ti in range(nn):
                        k.mm(ps[:, ti * 128:(ti + 1) * 128], u_all[:, cl, (t4 + ti) * 128:(t4 + ti + 1) * 128], P["ident"],
                             True, True, reads=[("u", bi, cl), "ident"], writes=[("ps", b)], tr=True)
                    k.copy(u_tok[:, t4:t4 + nn, cl * 128:(cl + 1) * 128],
                           ps[:, 0:nn * 128].rearrange("p (t c) -> p t c", c=128), reads=[("ps", b)],
                           writes=[("u_tok", bi, cl)], eng="act")

        def stage_b():
            for fa in range(NT):
                ba, bb_ = 2, 3
                if fa % 2:
                    ba, bb_ = 4, 5
                psa = k.ps[ba]
                psb = k.ps[bb_]
                for ti in range(NT):
                    k.mm(psa, F[:, ti, fa * 128:(fa + 1) * 128], u_tok[:, ti, :], ti == 0, ti == NT - 1,
                         reads=["F"] + utk, writes=[("ps", ba)])
                for ti in range(NT):
                    k.mm(psb, F[:, ti, L + fa * 128:L + (fa + 1) * 128], u_tok[:, ti, :], ti == 0, ti == NT - 1,
                         reads=["F"] + utk, writes=[("ps", bb_)])
                tp_ = fa % NBUF
                tq = tqb[tp_]
                k.tt(tq[0], psa, KA[:, fa, cs], ALU.mult, reads=[("ps", ba), ("KA", fa)], writes=[("tq", tp_, 0)])
                k.tt(tq[1], psb, KB[:, fa, cs], ALU.mult, reads=[("ps", bb_), ("KB", fa)], writes=[("tq", tp_, 1)])
                k.tt(tq[2], psa, KB[:, fa, cs], ALU.mult, reads=[("ps", ba), ("KB", fa)], writes=[("tq", tp_, 2)])
                k.tt(tq[3], psb, KA[:, fa, cs], ALU.mult, reads=[("ps", bb_), ("KA", fa)], writes=[("tq", tp_, 3)])
                k.tt(Y[:, fa, :], tq[0], tq[1], ALU.subtract, reads=[("tq", tp_, 0), ("tq", tp_, 1)], writes=[("Y", bi, fa)],
                     eng="pool")
                k.tt(Y[:, NT + fa, :], tq[2], tq[3], ALU.add, reads=[("tq", tp_, 2), ("tq", tp_, 3)],
                     writes=[("Y", bi, NT + fa)], eng="pool")
                if fa == 0:
                    k.copy(Y[0:1, 0, :], tq[0][0:1, :], reads=[("tq", tp_, 0), ("Y", bi, 0)], writes=[("Y", bi, 0)], eng="pool")
                    k.copy(Y[0:1, NT, :], tq[1][0:1, :], reads=[("tq", tp_, 1), ("Y", bi, NT)], writes=[("Y", bi, NT)],
                           eng="pool")

        def stage_c():
            for cl in range(NCC):
                cc = half * NCC + cl
                oi = cnt["nbo"] % 2
                cnt["nbo"] += 1
                for p0 in range(0, L, 512):
                    pn = min(512, L - p0)
                    b = 6 + (cnt["nps"] % 2)
                    cnt["nps"] += 1
                    ps = k.ps[b]
                    for f in range(NF):
                        k.mm(ps[:, 0:pn], Y[:, f, cl * 128:(cl + 1) * 128], FT[:, f, p0:p0 + pn], f == 0, f == NF - 1,
                             reads=ykeys + ["FT"], writes=[("ps", b)])
                    e_ = ep[(p0 // 512) % 2]
                    k.stt(e_[:, 0:pn], u_all[:, cl, p0:p0 + pn], db[:, cc:cc + 1], ps[:, 0:pn], ALU.mult, ALU.add,
                          reads=[("u", bi, cl), "db", ("ps", b)], writes=[("ep", (p0 // 512) % 2)])
                    k.tt(bo[oi][:, p0:p0 + pn], e_[:, 0:pn], x0_all[:, cl, p0:p0 + pn], ALU.mult,
                         reads=[("ep", (p0 // 512) % 2), ("x0", bi, cl)], writes=[("bo", oi)], eng="pool")
                k.dma(mixT_d[1024 + cc * 128:1024 + (cc + 1) * 128, t0:t0 + L], bo[oi], reads=[("bo", oi)],
                      writes=[("mixT_d", "hy", cc, t0)])

        return stage_a, stage_b, stage_c

    units = []
    for t0 in seq_cols:
        for half in range(HYW // CH):
            units.append(make_unit(len(units), t0, half))
    if NBUF == 2:
        units[0][0]()
        for n_, (_, sb_fn, sc_fn) in enumerate(units):
            if n_ + 1 < len(units):
                units[n_ + 1][0]()
            sb_fn()
            sc_fn()
    else:
        for (sa_fn, sb_fn, sc_fn) in units:
            sa_fn()
            sb_fn()
            sc_fn()
    k.s.barrier()
    A.off = mark


_CONST_CACHE = {}


def prep_core_inputs_l0(inp, core, d):
    b = core // 4
    d["w_in_even"] = inp["w_in_even"]
    d["kcT"] = np.ascontiguousarray(inp["cache_k"][b, 0].reshape(PAST, 1024).T)
    d["cv"] = np.ascontiguousarray(inp["cache_v"][b, 0].reshape(PAST, 1024))
    d["natt"] = natt_table(inp["na_rpb"][0])
    for L in (256, 1024):
        if L not in _CONST_CACHE:
            _CONST_CACHE[L] = hyena_consts(L)
        c = _CONST_CACHE[L]
        d[f"hy_zT{L}"] = c["zT"]
        d[f"hy_win{L}"] = c["window"]
        d[f"hy_F{L}"] = c["F"]
        d[f"hy_FT{L}"] = c["FT"]
    d["hy_w1"] = inp["hy_filt_w1"][0]
    d["hy_w2"] = inp["hy_filt_w2"][0]
    d["hy_w3"] = inp["hy_filt_w3"][0]
    d["hy_fvec"] = np.ascontiguousarray(np.stack([inp["hy_filt_b1"][0], inp["hy_filt_freq"][0], inp["hy_filt_b2"][0]], axis=1))
    sw = inp["hy_short_w"][0]
    d["hy_swT"] = np.ascontiguousarray(np.transpose(sw.reshape(3, 24, 128), (2, 1, 0)))
    d["hy_sbT"] = pk(inp["hy_short_b"][0])
    d["hy_dbT"] = pk(inp["hy_bias_d"][0])
    return d


def phase_outproj(k, w_dram, src_x, dst_x, blocks, l):
    A = k.arena
    mark = A.off
    mixT_d = k.scratch("mixT_d", [2048, 2048], BF16)
    wv = w_dram.rearrange("(k p) c -> p k c", p=128)
    W = A.alloc([16, 2048], BF16)
    for cb in range(4):
        k.dma(W[:, :, cb * 512:(cb + 1) * 512], wv[:, :, cb * 512:(cb + 1) * 512], writes=[("W", cb)], q="pool")
    mv = mixT_d.rearrange("(k p) t -> p k t", p=128)
    xv = src_x.rearrange("(k p) t -> p k t", p=128)
    ov = dst_x.rearrange("(k p) t -> p k t", p=128)
    m_rows = mixT_d.rearrange("d (b t) -> (d b) t", t=256)
    x_rows = src_x.rearrange("d (b t) -> (d b) t", t=256)
    mb = [A.alloc([16, 512], BF16) for _ in range(2)]
    xb = [A.alloc([16, 512]) for _ in range(2)]
    nps = 0
    for bi, (src0, n, j, dst0) in enumerate(blocks):
        i = bi % 2
        if src0 is None:
            k.s.rec("pool", None, writes=[("mb", i), ("xb", i)])
            for kk in range(16):
                k.gather(mb[i][:, kk, 0:n], m_rows, k.P["own_idx"][:, kk:kk + 1], reads=["own_idx", ("mb", i)],
                         writes=[("mbg", i, kk)])
                k.gather(xb[i][:, kk, 0:n], x_rows, k.P["own_idx"][:, kk:kk + 1], reads=["own_idx", ("xb", i)],
                         writes=[("xbg", i, kk)])
        else:
            k.dma(mb[i][:, :, 0:n], mv[:, :, src0:src0 + n], writes=[("mb", i)])
            k.dma(xb[i][:, :, 0:n], xv[:, :, src0:src0 + n], writes=[("xb", i)])
        _, _, G_ = mod_views(k, l, 0, j)
        for dch in range(16):
            b = nps % 4
            nps += 1
            ps = k.ps[b]
            for kk in range(16):
                k.mm(ps[:, 0:n], W[:, kk, dch * 128:(dch + 1) * 128], mb[i][:, kk, 0:n], kk == 0, kk == 15,
                     reads=[("W", dch // 4), ("mb", i), ("mbg", i, kk)], writes=[("ps", b)])
            k.stt(xb[i][:, dch, 0:n], ps[:, 0:n], G_[:, dch:dch + 1], xb[i][:, dch, 0:n], ALU.mult, ALU.add,
                  reads=[("ps", b), ("xb", i), ("xbg", i, dch), ("modT", l, j)], writes=[("xb", i)])
        k.dma(ov[:, :, dst0:dst0 + n], xb[i][:, :, 0:n], reads=[("xb", i)], writes=[("xout", bi)], q="pool")
    k.s.barrier()
    A.off = mark


def bc(ap, shape, axis):
    return ap.unsqueeze(axis).to_broadcast(list(shape))


def phase_moe(k, src_x, dst_x, groups, l):
    A = k.arena
    P = k.P
    w1_d = k.inp("moe_w1", [2, NEXP, D, FF])
    w3_d = k.inp("moe_w3", [2, NEXP, D, FF])
    w2_d = k.inp("moe_w2", [2, NEXP, FF, D])
    wr_d = k.inp("moe_wrT", [128, 2, 16, 20])
    br_d = k.inp("moe_brB", [128, 2, 20])
    sel_d = k.inp("moe_sel", [16, 16, 128])
    xv = src_x.rearrange("(k p) t -> p k t", p=128)
    ov = dst_x.rearrange("(k p) t -> p k t", p=128)
    for g, (c0, TG, j) in enumerate(groups):
        mark = A.off
        halves = [(o, min(512, TG - o)) for o in range(0, TG, 512)]
        jh = [j] * len(halves) if isinstance(j, int) else list(j)
        NTI = TG // 128
        xacc = A.alloc([16, TG])
        hT = A.alloc([16, TG], BF16)
        combT = A.alloc([TG])
        sel = A.alloc([16, 128])
        k.dma(sel[0:16], sel_d, writes=["sel"])
        for hb, (o, n) in enumerate(halves):
            k.dma(xacc[:, :, o:o + n], xv[:, :, c0 + o:c0 + o + n], writes=[("xacc", hb)])
        m1 = A.off
        sq = A.alloc([16, 512], BF16)
        rs = A.alloc([512])
        tmp = [A.alloc([512]) for _ in range(2)]
        hf = A.alloc([16, 512])
        wr = A.alloc([16, 20])
        br = A.alloc([20])
        lg = A.alloc([NTI, 20])
        lgT = A.alloc([512])
        k.dma(wr, wr_d[:, l], writes=["wr"])
        k.dma(br, br_d[:, l], writes=["br"])
        psl = k.ps[7]
        for hb, (o, n) in enumerate(halves):
            hcs = slice(o, o + n)
            j = jh[hb]
            A_, B_, G_ = mod_views(k, l, 1, j)
            k.act(sq[:, :, 0:n], xacc[:, :, hcs], AF.Square, reads=[("xacc", hb)], writes=["sq"])
            ps = k.ps[hb]
            for kk in range(16):
                k.mm(ps[:, 0:n], P["ones_bf"], sq[:, kk, 0:n], kk == 0, kk == 15, reads=["sq", "ones"], writes=[("ps", hb)])
            k.act(rs[:, 0:n], ps[:, 0:n], AF.Sqrt, bias=P["eps"], scale=1.0 / D, reads=[("ps", hb), "eps"], writes=["rs"])
            k.recip(rs[:, 0:n], rs[:, 0:n], reads=["rs"], writes=["rs"])
            k.tt(hf[:, :, 0:n], xacc[:, :, hcs], rs[:, 0:n].unsqueeze(1).to_broadcast([128, 16, n]), ALU.mult,
                 reads=[("xacc", hb), "rs"], writes=["hf"] + [("hfk", kk) for kk in range(16)])
            for kk in range(16):
                k.act(hf[:, kk, 0:n], hf[:, kk, 0:n], AF.Identity, bias=B_[:, kk:kk + 1], scale=A_[:, kk:kk + 1],
                      reads=["hf", ("modT", l, j), ("Amod", l, 1, j)], writes=[("hfk", kk)])
            k.copy(hT[:, :, hcs], hf[:, :, 0:n], reads=["hf"] + [("hfk", kk) for kk in range(16)], writes=[("hT", hb)],
                   eng="dve")
            psr = k.ps[6]
            for kk in range(16):
                k.mm(psr[0:20, 0:n], wr[:, kk, :], hf[:, kk, 0:n], kk == 0, kk == 15,
                     reads=["hf", ("hfk", kk), "wr"], writes=[("ps", 6)])
            k.copy(lgT[0:20, 0:n], psr[0:20, 0:n], reads=[("ps", 6)], writes=["lgT"], eng="dve")
            for t4 in range(n // 128):
                ti = o // 128 + t4
                k.mm(psl[:, ti * 20:(ti + 1) * 20], lgT[0:20, t4 * 128:(t4 + 1) * 128], P["ident"][0:20, 0:20], True, True,
                     reads=["lgT", "ident"], writes=[("ps", 7)], tr=True)
        r_ = {}
        for nm, shp in (("gmax", [NTI]), ("ge", [NTI, 4]), ("gs", [NTI]), ("gp", [NTI]), ("goh", [NTI, 4]),
                        ("t44", [NTI, 4, 4]), ("esel", [NTI, 4]), ("m1", [NTI]), ("oh1", [NTI, 4]), ("e2", [NTI, 4]),
                        ("m2", [NTI]), ("oh2", [NTI, 4]), ("dd", [NTI]), ("w1", [NTI]), ("w2", [NTI]), ("ws", [NTI, 4]),
                        ("ws2", [NTI, 4]), ("comb", [NTI, 4, 4])):
            r_[nm] = A.alloc(shp)
        RK = "route"
        k.tt(lg, psl[:, 0:NTI * 20].rearrange("p (t e) -> p t e", e=20), bc(br, [128, NTI, 20], 1), ALU.add,
             reads=[("ps", 7), "br"], writes=[RK])

        def dv(fn):
            k.s.rec("dve", fn, reads=[RK], writes=[RK])

        def route(r_, lg, NTI):
            gl = lg[:, :, 0:4]
            el = lg[:, :, 4:20].rearrange("p t (g j) -> p t g j", j=4)
            s3 = [128, NTI, 4]
            s4 = [128, NTI, 4, 4]
            dv(lambda e: e.tensor_reduce(out=r_["gmax"], in_=gl, axis=AX.X, op=ALU.max))
            dv(lambda e: e.tensor_tensor(out=r_["ge"], in0=gl, in1=bc(r_["gmax"], s3, 2), op=ALU.subtract))
            k.s.rec("act", lambda e: e.activation(out=r_["ge"], in_=r_["ge"], func=AF.Exp), reads=[RK], writes=[RK])
            dv(lambda e: e.tensor_reduce(out=r_["gs"], in_=r_["ge"], axis=AX.X, op=ALU.add))
            dv(lambda e: e.reciprocal(out=r_["gp"], in_=r_["gs"]))
            dv(lambda e: e.tensor_tensor(out=r_["goh"], in0=gl, in1=bc(r_["gmax"], s3, 2), op=ALU.is_ge))
            dv(lambda e: e.tensor_tensor(out=r_["t44"], in0=el, in1=bc(r_["goh"], s4, 3), op=ALU.mult))
            dv(lambda e: e.tensor_reduce(out=r_["esel"], in_=r_["t44"].rearrange("p t g j -> p t j g"), axis=AX.X, op=ALU.add))
            dv(lambda e: e.tensor_reduce(out=r_["m1"], in_=r_["esel"], axis=AX.X, op=ALU.max))
            dv(lambda e: e.tensor_tensor(out=r_["oh1"], in0=r_["esel"], in1=bc(r_["m1"], s3, 2), op=ALU.is_ge))
            dv(lambda e: e.scalar_tensor_tensor(out=r_["e2"], in0=r_["oh1"], scalar=-1e30, in1=r_["esel"], op0=ALU.mult, op1=ALU.add))
            dv(lambda e: e.tensor_reduce(out=r_["m2"], in_=r_["e2"], axis=AX.X, op=ALU.max))
            dv(lambda e: e.tensor_tensor(out=r_["oh2"], in0=r_["e2"], in1=bc(r_["m2"], s3, 2), op=ALU.is_ge))
            dv(lambda e: e.tensor_tensor(out=r_["dd"], in0=r_["m2"], in1=r_["m1"], op=ALU.subtract))
            k.s.rec("act", lambda e: e.activation(out=r_["dd"], in_=r_["dd"], func=AF.Exp), reads=[RK], writes=[RK])
            dv(lambda e: e.tensor_scalar(out=r_["dd"], in0=r_["dd"], scalar1=1.0, scalar2=None, op0=ALU.add))
            dv(lambda e: e.reciprocal(out=r_["w1"], in_=r_["dd"]))
            dv(lambda e: e.tensor_tensor(out=r_["w1"], in0=r_["w1"], in1=r_["gp"], op=ALU.mult))
            dv(lambda e: e.tensor_tensor(out=r_["w2"], in0=r_["gp"], in1=r_["w1"], op=ALU.subtract))
            dv(lambda e: e.tensor_tensor(out=r_["ws"], in0=r_["oh1"], in1=bc(r_["w1"], s3, 2), op=ALU.mult))
            dv(lambda e: e.tensor_tensor(out=r_["ws2"], in0=r_["oh2"], in1=bc(r_["w2"], s3, 2), op=ALU.mult))
            dv(lambda e: e.tensor_tensor(out=r_["ws"], in0=r_["ws"], in1=r_["ws2"], op=ALU.add))
            dv(lambda e: e.tensor_tensor(out=r_["comb"], in0=bc(r_["goh"], s4, 3), in1=bc(r_["ws"], s4, 2), op=ALU.mult))

        route(r_, lg, NTI)
        comb = r_["comb"].rearrange("p t g j -> p t (g j)")
        for hb, (o, n) in enumerate(halves):
            ps = k.ps[hb]
            for t4 in range(n // 128):
                k.mm(ps[0:16, t4 * 128:(t4 + 1) * 128], comb[:, o // 128 + t4, :], P["ident"], True, True,
                     reads=[RK, "ident"], writes=[("ps", hb)], tr=True)
            k.copy(combT[0:16, o:o + n], ps[0:16, 0:n], reads=[("ps", hb)], writes=["combT"])
        k.s.barrier()
        A.off = m1
        NQ = 6
        wq = [A.alloc([16, 128], BF16) for _ in range(NQ)]
        w2q = [A.alloc([4, 512], BF16) for _ in range(4)]
        hid1 = A.alloc([4, TG], BF16)
        hid = [hid1, hid1]
        cb1 = A.alloc([TG])
        cb = [cb1, cb1]
        sl = [A.alloc([512]) for _ in range(2)]
        t3 = [A.alloc([512]) for _ in range(2)]
        nq = 0
        nst = 0
        nps = 0
        for e in range(NEXP):
            ei = e % 2
            w1v = w1_d[l, e].rearrange("(k p) f -> p k f", p=128)
            w3v = w3_d[l, e].rearrange("(k p) f -> p k f", p=128)
            ei = 0
            w2v = w2_d[l, e].rearrange("(c p) d -> p c d", p=128)
            for hb, (o, n) in enumerate(halves):
                b = 6 + hb % 2
                k.mm(k.ps[b][:, 0:n], sel[0:16, e, :], combT[0:16, o:o + n], True, True, reads=["sel", "combT"],
                     writes=[("ps", b)])
                k.copy(cb[ei][:, o:o + n], k.ps[b][:, 0:n], reads=[("ps", b)], writes=[("cb", ei, hb)], eng="act")
            for fq in range(4):
                q1 = nq % NQ
                q3 = (nq + 1) % NQ
                nq += 2
                k.dma(wq[q1], w1v[:, :, fq * 128:(fq + 1) * 128], writes=[("wq", q1)], q="pool")
                k.dma(wq[q3], w3v[:, :, fq * 128:(fq + 1) * 128], writes=[("wq", q3)], q="pool")
                for hb, (o, n) in enumerate(halves):
                    hcs = slice(o, o + n)
                    b1 = (nps % 2)
                    b3 = 2 + (nps % 2)
                    nps += 1
                    for kk in range(16):
                        k.mm(k.ps[b1][:, 0:n], wq[q1][:, kk, :], hT[:, kk, hcs], kk == 0, kk == 15,
                             reads=[("wq", q1), ("hT", hb)], writes=[("ps", b1)])
                    for kk in range(16):
                        k.mm(k.ps[b3][:, 0:n], wq[q3][:, kk, :], hT[:, kk, hcs], kk == 0, kk == 15,
                             reads=[("wq", q3), ("hT", hb)], writes=[("ps", b3)])
                    si = nst % 2
                    nst += 1
                    k.act(sl[si][:, 0:n], k.ps[b1][:, 0:n], AF.Silu, reads=[("ps", b1)], writes=[("sl", si)])
                    k.tt(t3[si][:, 0:n], k.ps[b3][:, 0:n], cb[ei][:, hcs], ALU.mult, reads=[("ps", b3), ("cb", ei, hb)],
                         writes=[("t3", si)])
                    k.tt(hid[ei][:, fq, hcs], sl[si][:, 0:n], t3[si][:, 0:n], ALU.mult, reads=[("sl", si), ("t3", si)],
                         writes=[("hid", ei, hb)], eng="dve")
            for dch in range(16):
                dq = dch // 4
                if dch % 4 == 0:
                    k.dma(w2q[dq], w2v[:, :, dq * 512:(dq + 1) * 512], writes=[("w2q", dq)], q="pool")
                for hb, (o, n) in enumerate(halves):
                    hcs = slice(o, o + n)
                    j = jh[hb]
                    A_, B_, G_ = mod_views(k, l, 1, j)
                    b = 4 + (nps % 2)
                    nps += 1
                    for fq in range(4):
                        k.mm(k.ps[b][:, 0:n], w2q[dq][:, fq, (dch % 4) * 128:(dch % 4 + 1) * 128], hid[ei][:, fq, hcs],
                             fq == 0, fq == 3, reads=[("w2q", dq), ("hid", ei, hb)], writes=[("ps", b)])
                    k.stt(xacc[:, dch, hcs], k.ps[b][:, 0:n], G_[:, dch:dch + 1], xacc[:, dch, hcs], ALU.mult, ALU.add,
                          reads=[("ps", b), ("xacc", hb), ("modT", l, j)], writes=[("xacc", hb)])
        for hb, (o, n) in enumerate(halves):
            k.dma(ov[:, :, c0 + o:c0 + o + n], xacc[:, :, o:o + n], reads=[("xacc", hb)], writes=[("xout", g, hb)])
        k.s.barrier()
        A.off = mark


def odd_consts():
    c = np.arange(256, dtype=np.float64)
    ang = 2.0 * math.pi * np.outer(c, c) / 256.0
    CS = np.concatenate([np.cos(ang), np.sin(ang)], axis=1)
    d = {"od_CS": CS.astype(ml_dtypes.bfloat16)}
    for L in (256, 1024):
        l_ = np.arange(L, dtype=np.float64)
        a = 2.0 * math.pi * np.outer(l_, l_) / L
        d[f"od_CL{L}"] = np.stack([np.cos(a), -np.sin(a)], axis=1).astype(ml_dtypes.bfloat16)
        pos = np.arange(L)
        inv = np.zeros((4, L), np.float32)
        for g, w in enumerate((2, 4, 8, 16)):
            lo = np.clip(pos - w // 2, 0, L)
            hi = np.clip(pos + w // 2, 0, L)
            inv[g] = 1.0 / (hi - lo).astype(np.float32)
        d[f"od_inv{L}"] = np.ascontiguousarray(np.broadcast_to(inv[None], (128, 4, L)))
    return d


def phase_odd(k, src_x, T, cond_of_tb):
    A = k.arena
    mark = A.off
    w_in = k.inp("w_in_odd", [1, D, D])
    wv = w_in[0].rearrange("(k p) c -> p k c", p=128)
    top = 16 * T // 2
    A.words -= top
    pT = A.h[:, A.words:A.words + top].bitcast(BF16).rearrange("p (a b) -> p a b", a=16)
    m0 = A.off
    hT = A.alloc([16, T], BF16)
    phase_prenorm(k, src_x, T, 1, 0, cond_of_tb, hT, "hT", nbuf=2, bw=256)
    hkeys = [[("hT", tb, kk) for kk in range(16)] for tb in range(T // 512)]
    wb = [A.alloc([16, 512], BF16) for _ in range(2)]
    nps = 0
    for cb in range(4):
        i = cb % 2
        k.dma(wb[i], wv[:, :, cb * 512:(cb + 1) * 512], writes=[("wb", i)], q="pool")
        for fc in range(4):
            ch = cb * 4 + fc
            for tb in range(T // 512):
                b = nps % 4
                nps += 1
                for kk in range(16):
                    k.mm(k.ps[b], wb[i][:, kk, fc * 128:(fc + 1) * 128], hT[:, kk, tb * 512:(tb + 1) * 512], kk == 0, kk == 15,
                         reads=[("wb", i), hkeys[tb][kk]], writes=[("ps", b)])
                k.copy(pT[:, ch, tb * 512:(tb + 1) * 512], k.ps[b], reads=[("ps", b)], writes=[("pT", ch)],
                       eng="act" if nps % 2 else "dve")
    k.s.barrier()
    A.off = m0
    phase_odd2(k, pT, T, mark)
    A.words += top


def phase_odd2(k, pT, T, mark):
    A = k.arena
    mixT_d = k.scratch("mixT_d", [2048, 2048], BF16)
    fn_d = k.inp("fn_lin", [1, 4, 256, 256])
    pl_d = k.inp("pool_lin", [1, 4, 256, 256])
    psc_d = k.inp("od_pscT", [128, 8])
    CS_d = k.inp("od_CS", [256, 512], BF16)
    CS = A.alloc([2, 512], BF16)
    k.dma(CS, CS_d.rearrange("(c p) f -> p c f", p=128), writes=["CS"])
    lin = A.alloc([2, 4, 2, 256], BF16)
    for g_ in range(4):
        k.dma(lin[:, 0, g_], fn_d[0, g_].rearrange("(c p) d -> p c d", p=128), writes=[("lin0", g_)], q="pool")
        k.dma(lin[:, 1, g_], pl_d[0, g_].rearrange("(c p) d -> p c d", p=128), writes=[("lin1", g_)], q="pool")
    psc = A.alloc([8])
    k.dma(psc, psc_d, writes=["psc"])
    seqs = [(256, s * 256) for s in range(4)] + [(1024, 1024)]
    cur_L = None
    mL = A.off
    nps = 0
    for (L, t0) in seqs:
        NT = L // 128
        if L != cur_L:
            k.s.barrier()
            A.off = mL
            cur_L = L
            CL_d = k.inp(f"od_CL{L}", [L, 2, L], BF16)
            inv_d = k.inp(f"od_inv{L}", [128, 4, L])
            CL = A.alloc([NT, 2, L], BF16)
            k.dma(CL, CL_d.rearrange("(n p) s l -> p n s l", p=128), writes=["CL"])
            inv = A.alloc([4, L])
            k.dma(inv, inv_d, writes=["inv"])
            ucs = A.alloc([NT, 512], BF16)
            fT = A.alloc([8, L], BF16)
            pm = A.alloc([8, L], BF16)
            pad = [A.alloc([L + 16]) for _ in range(2)]
            sa = [A.alloc([L + 16]) for _ in range(2)]
            sb_ = [A.alloc([L + 16]) for _ in range(2)]
            ob = [A.alloc([L], BF16) for _ in range(2)]
            for i in range(2):
                k.memset(pad[i], 0.0, writes=[("pad", i)])
        scale = 1.0 / math.sqrt(256.0 * L)
        for g in range(4):
            for ti in range(NT):
                b = nps % 4
                nps += 1
                for ch in range(2):
                    k.mm(k.ps[b], pT[:, g * 2 + ch, t0 + ti * 128:t0 + (ti + 1) * 128], CS[:, ch, :], ch == 0, ch == 1,
                         reads=[("pT", g * 2 + ch), "CS"], writes=[("ps", b)])
                k.copy(ucs[:, ti, :], k.ps[b], reads=[("ps", b)], writes=[("ucs", ti)], eng="act" if nps % 2 else "dve")
            ukeys = [("ucs", ti) for ti in range(NT)]
            for cc in range(2):
                for p0 in range(0, L, 512):
                    pn = min(512, L - p0)
                    b = nps % 4
                    nps += 1
                    n = 0
                    for s_ in range(2):
                        for ti in range(NT):
                            k.mm(k.ps[b][:, 0:pn], ucs[:, ti, s_ * 256 + cc * 128:s_ * 256 + (cc + 1) * 128],
                                 CL[:, ti, s_, p0:p0 + pn], n == 0, n == 2 * NT - 1, reads=ukeys + ["CL"], writes=[("ps", b)])
                            n += 1
                    k.act(fT[:, g * 2 + cc, p0:p0 + pn], k.ps[b][:, 0:pn], AF.Copy, scale=scale, reads=[("ps", b)],
                          writes=[("fT", g * 2 + cc)])
        for c8 in range(8):
            g = c8 // 2
            w = (2, 4, 8, 16)[g]
            i = c8 % 2
            e1 = "pool" if c8 % 2 else "dve"
            k.copy(pad[i][:, 8:8 + L], pT[:, 8 + c8, t0:t0 + L], reads=[("pT", 8 + c8)], writes=[("pad", i)], eng=e1)
            W_ = L + 16
            k.tt(sa[i][:, 0:W_ - 1], pad[i][:, 0:W_ - 1], pad[i][:, 1:W_], ALU.add, reads=[("pad", i)], writes=[("sa", i)], eng=e1)
            cur, curk, oth, othk, span = sa[i], ("sa", i), sb_[i], ("sb", i), 2
            while span < w:
                k.tt(oth[:, 0:W_ - 2 * span + 1], cur[:, 0:W_ - 2 * span + 1], cur[:, span:W_ - span + 1], ALU.add,
                     reads=[curk], writes=[othk], eng=e1)
                cur, curk, oth, othk = oth, othk, cur, curk
                span *= 2
            o0 = 8 - w // 2
            k.tt(oth[:, 0:L], cur[:, o0:o0 + L], inv[:, g, :], ALU.mult, reads=[curk, "inv"], writes=[othk], eng=e1)
            k.tt(pm[:, c8, :], oth[:, 0:L], pad[i][:, 8:8 + L], ALU.subtract, reads=[othk, ("pad", i)], writes=[("pm", c8)], eng=e1)
        no = 0
        for which, src, skey in ((0, fT, "fT"), (1, pm, "pm")):
            for g in range(4):
                for dc in range(2):
                    oi = no % 2
                    no += 1
                    for p0 in range(0, L, 512):
                        pn = min(512, L - p0)
                        b = nps % 4
                        nps += 1
                        for cc in range(2):
                            k.mm(k.ps[b][:, 0:pn], lin[:, which, g, cc, dc * 128:(dc + 1) * 128], src[:, g * 2 + cc, p0:p0 + pn],
                                 cc == 0, cc == 1, reads=[(f"lin{which}", g), (skey, g * 2), (skey, g * 2 + 1)], writes=[("ps", b)])
                        if which == 0:
                            k.copy(ob[oi][:, p0:p0 + pn], k.ps[b][:, 0:pn], reads=[("ps", b)], writes=[("ob", oi)], eng="act")
                        else:
                            k.ts(ob[oi][:, p0:p0 + pn], k.ps[b][:, 0:pn], psc[:, g * 2 + dc:g * 2 + dc + 1], ALU.mult,
                                 reads=[("ps", b), "psc"], writes=[("ob", oi)])
                    row = which * 1024 + (g * 2 + dc) * 128
                    k.dma(mixT_d[row:row + 128, t0:t0 + L], ob[oi], reads=[("ob", oi)], writes=[("mixT_d", row, t0)])
    k.s.barrier()
    A.off = mark


def phase_final(k, src_x, T):
    A = k.arena
    P = k.P
    mark = A.off
    yT = k.out("yT", [D, T])
    xv = src_x.rearrange("(k p) t -> p k t", p=128)
    ov = yT.rearrange("(k p) t -> p k t", p=128)
    xb = [A.alloc([16, 512]) for _ in range(2)]
    sq = A.alloc([16, 512], BF16)
    rs = A.alloc([512])
    nf = P["normT"][:, 4, :]
    for tb, o in enumerate(range(0, T, 512)):
        n = min(512, T - o)
        b = tb % 2
        cs = slice(o, o + n)
        k.dma(xb[b][:, :, 0:n], xv[:, :, cs], writes=[("xb", b)])
        k.act(sq[:, :, 0:n], xb[b][:, :, 0:n], AF.Square, reads=[("xb", b)], writes=["sq"])
        ps = k.ps[b]
        for kk in range(16):
            k.mm(ps[:, 0:n], P["ones_bf"], sq[:, kk, 0:n], kk == 0, kk == 15, reads=["sq", "ones"], writes=[("ps", b)])
        k.act(rs[:, 0:n], ps[:, 0:n], AF.Sqrt, bias=P["eps"], scale=1.0 / D, reads=[("ps", b), "eps"], writes=["rs"])
        k.recip(rs[:, 0:n], rs[:, 0:n], reads=["rs"], writes=["rs"])
        k.tt(xb[b][:, :, 0:n], xb[b][:, :, 0:n], rs[:, 0:n].unsqueeze(1).to_broadcast([128, 16, n]), ALU.mult,
             reads=[("xb", b), "rs"], writes=[("xb", b)])
        k.tt(xb[b][:, :, 0:n], xb[b][:, :, 0:n], nf.unsqueeze(2).to_broadcast([128, 16, n]), ALU.mult,
             reads=[("xb", b), "normT"], writes=[("xb", b)])
        k.dma(ov[:, :, cs], xb[b][:, :, 0:n], reads=[("xb", b)], writes=[("yout", tb)], q="pool")
    k.s.barrier()
    A.off = mark


def build_full(dbg=False):
    k = new_builder(dbg)
    T = 2048
    mark = k.arena.off
    phase_mod(k)
    xT = k.inp("xT", [D, T])
    cond = lambda tb: 0 if tb < 2 else 1
    hT = k.arena.alloc([16, T], BF16)
    phase_prenorm(k, xT, T, 0, 0, cond, hT, "hT")
    phase_inproj0(k, hT, T)
    k.arena.off = mark
    phase_attn_prompt(k, T)
    phase_attn_sample(k, T)
    phase_hyena(k, 256, [0, 256, 512, 768], T, "p")
    phase_hyena(k, 1024, [1024], T, "s")
    T1 = 1280
    x1 = k.scratch("x1T", [D, T])
    x2 = k.scratch("x2T", [D, T])
    x3 = k.scratch("x3T", [D, T1])
    x4 = k.scratch("x4T", [D, T1])
    w_oe = k.inp("w_out_even", [1, D, D])
    w_oo = k.inp("w_out_odd", [1, D, D])
    oi = k.inp("own_idx", [128, 16], I32)
    k.dma(k.P["own_idx"], oi, writes=["own_idx"])
    blk0 = [(tb * 512, 512, cond(tb), tb * 512) for tb in range(4)]
    phase_outproj(k, w_oe[0], xT, x1, blk0, 0)
    phase_moe(k, x1, x2, [(0, 1024, 0), (1024, 1024, 1)], 0)
    phase_odd(k, x2, T, cond)
    blk1 = [(0, 512, 0, 0), (512, 512, 0, 512), (None, 256, 1, 1024)]
    phase_outproj(k, w_oo[0], x2, x3, blk1, 1)
    phase_moe(k, x3, x4, [(0, 1280, (0, 0, 1))], 1)
    phase_final(k, x4, T1)
    nc = finish(k)
    return nc, k


_SHARED = {}


def prep_shared(inp):
    d = {}
    for n in ("w_out_even", "w_out_odd", "w_in_odd", "fn_lin", "pool_lin", "moe_w1", "moe_w3", "moe_w2"):
        d[n] = inp[n]
    wr = np.concatenate([inp["moe_w_group"], inp["moe_w_expert"]], axis=-1)
    d["moe_wrT"] = np.ascontiguousarray(np.transpose(wr.reshape(2, 16, 128, 20), (2, 0, 1, 3)))
    br = np.concatenate([inp["moe_b_group"], inp["moe_b_expert"]], axis=-1)
    d["moe_brB"] = np.ascontiguousarray(np.broadcast_to(br[None], (128, 2, 20)))
    sel = np.zeros((16, 16, 128), np.float32)
    for e in range(16):
        sel[e, e, :] = 1.0
    d["moe_sel"] = sel
    if "od" not in _CONST_CACHE:
        _CONST_CACHE["od"] = odd_consts()
    d.update(_CONST_CACHE["od"])
    d["od_pscT"] = pk(inp["pool_scale"][0])
    return d


def kernel(**inputs):
    inp = {k_: np.asarray(v) for k_, v in inputs.items()}
    nc, k = build_full(False)
    shared = prep_shared(inp)
    in_maps = []
    for core in range(NCORES):
        ci = prep_core_inputs(inp, core)
        prep_core_inputs_l0(inp, core, ci)
        ci.update(shared)
        in_maps.append({n: ci[n] for n in k.inputs})
    res = run_bass_kernel_spmd(nc, in_maps, core_ids=list(range(NCORES)))
    y_p = np.zeros((32, SEQ, D), np.float32)
    y_s = np.zeros((2, DSEQ, D), np.float32)
    nk = np.zeros((32, 1, SEQ, NH, HD), np.float32)
    nv = np.zeros((32, 1, SEQ, NH, HD), np.float32)
    for core in range(NCORES):
        r = res.results[core]
        b, q = core // 4, core % 4
        yT = r["yT"]
        y_p[core * 4:(core + 1) * 4] = yT[:, 0:1024].T.reshape(4, SEQ, D)
        y_s[b, q * 256:(q + 1) * 256] = yT[:, 1024:1280].T
        nk[core * 4:(core + 1) * 4, 0] = r["nkT"].T.reshape(4, SEQ, NH, HD)
        nv[core * 4:(core + 1) * 4, 0] = r["nv"].reshape(4, SEQ, NH, HD)
    return y_p, y_s, nk, nv
```
